# Optimizing a Trainium2 kernel written in Bass

```python
import jax, jax.numpy as jnp
from jax import lax
import numpy as np

D_MODEL = 2048
BATCH = 4
SEQ = 2048
DEPTH = 1

HEAD_DIM = 128
N_HEADS_MOBA = 8
N_HEADS_SB = 8
D_MOBA = N_HEADS_MOBA * HEAD_DIM
D_SB = N_HEADS_SB * HEAD_DIM
D_MIX = D_MOBA + D_SB
D_QKV = 3 * D_MIX
D_FF = 4 * D_MODEL
MOBA_BLOCK = 256
MOBA_TOPK = 3
MOBA_Q_CHUNK = 16
SB_Q_BLOCK = 128
ROPE_THETA = 500000.0
ROPE_DIMS = HEAD_DIM // 4
EPS = 1e-6
NEG = -1e30

kernel_name = "hymba_moba_stickbreaking_sqrelu_block"


def rmsnorm(x, g):
    xf = x.astype(jnp.float32)
    y = xf * lax.rsqrt(jnp.mean(xf * xf, axis=-1, keepdims=True) + EPS)
    return (y * g.astype(jnp.float32)).astype(x.dtype)


def partial_rope(t, pos):
    half = ROPE_DIMS // 2
    inv_freq = ROPE_THETA ** (-jnp.arange(half, dtype=jnp.float32) / half)
    ang = pos.astype(jnp.float32)[:, None] * inv_freq[None, :]
    cos, sin = jnp.cos(ang), jnp.sin(ang)
    tr = t[..., :ROPE_DIMS].astype(jnp.float32)
    t1, t2 = tr[..., :half], tr[..., half:]
    rot = jnp.concatenate([t1 * cos - t2 * sin, t1 * sin + t2 * cos], axis=-1)
    return jnp.concatenate([rot.astype(t.dtype), t[..., ROPE_DIMS:]], axis=-1)


def moba_attention(q, k, v):
    B, H, S, Dh = q.shape
    n_blk = -(-S // MOBA_BLOCK)
    pad = n_blk * MOBA_BLOCK - S
    kb = jnp.pad(k, ((0, 0), (0, 0), (0, pad), (0, 0))).reshape(B, H, n_blk, MOBA_BLOCK, Dh)
    vb = jnp.pad(v, ((0, 0), (0, 0), (0, pad), (0, 0))).reshape(B, H, n_blk, MOBA_BLOCK, Dh)
    topk = min(MOBA_TOPK, n_blk)
    scale = Dh ** -0.5

    k_mean = jnp.mean(kb.astype(jnp.float32), axis=3)
    gate = jnp.einsum('bhsd,bhnd->bhsn', q.astype(jnp.float32), k_mean)
    q_blk = jnp.arange(S) // MOBA_BLOCK
    past = jnp.arange(n_blk)[None, :] < q_blk[:, None]
    gate = jnp.where(past, gate, NEG)
    _, sel = lax.top_k(gate, topk)

    n_chunk = S // MOBA_Q_CHUNK

    def to_chunks(a):
        return jnp.moveaxis(a.reshape(B, H, n_chunk, MOBA_Q_CHUNK, *a.shape[3:]), 2, 0)

    b_idx = jnp.arange(B)[:, None, None, None]
    h_idx = jnp.arange(H)[None, :, None, None]
    key_off = jnp.arange(MOBA_BLOCK)

    def chunk_fn(args):
        c, qc, selc = args
        q_pos = c * MOBA_Q_CHUNK + jnp.arange(MOBA_Q_CHUNK)
        blk = (c * MOBA_Q_CHUNK) // MOBA_BLOCK
        kg = kb[b_idx, h_idx, selc]
        vg = vb[b_idx, h_idx, selc]
        s_past = jnp.einsum('bhcd,bhcrkd->bhcrk', qc, kg,
                            preferred_element_type=jnp.float32) * scale
        slot_valid = jnp.arange(topk) < blk
        s_past = jnp.where(slot_valid[:, None], s_past, NEG)
        s_past = s_past.reshape(B, H, MOBA_Q_CHUNK, topk * MOBA_BLOCK)
        k_own = lax.dynamic_index_in_dim(kb, blk, axis=2, keepdims=False)
        v_own = lax.dynamic_index_in_dim(vb, blk, axis=2, keepdims=False)
        s_own = jnp.einsum('bhcd,bhkd->bhck', qc, k_own,
                           preferred_element_type=jnp.float32) * scale
        own_pos = blk * MOBA_BLOCK + key_off
        s_own = jnp.where(own_pos[None, :] <= q_pos[:, None], s_own, NEG)
        p = jax.nn.softmax(jnp.concatenate([s_past, s_own], axis=-1), axis=-1)
        p_past = p[..., :topk * MOBA_BLOCK].astype(v.dtype)
        p_own = p[..., topk * MOBA_BLOCK:].astype(v.dtype)
        o = jnp.einsum('bhck,bhckd->bhcd', p_past,
                       vg.reshape(B, H, MOBA_Q_CHUNK, topk * MOBA_BLOCK, Dh),
                       preferred_element_type=jnp.float32)
        o = o + jnp.einsum('bhck,bhkd->bhcd', p_own, v_own, preferred_element_type=jnp.float32)
        return o.astype(q.dtype)

    out = lax.map(chunk_fn, (jnp.arange(n_chunk), to_chunks(q), to_chunks(sel)))
    return jnp.moveaxis(out, 0, 2).reshape(B, H, S, Dh)


def stick_breaking_attention(q, k, v):
    B, H, S, Dh = q.shape
    n_qblk = S // SB_Q_BLOCK
    scale = Dh ** -0.5
    key_pos = jnp.arange(S)
    q_blocks = jnp.moveaxis(q.reshape(B, H, n_qblk, SB_Q_BLOCK, Dh), 2, 0)

    def block_fn(args):
        i, qb = args
        q_pos = i * SB_Q_BLOCK + jnp.arange(SB_Q_BLOCK)
        z = jnp.einsum('bhqd,bhkd->bhqk', qb, k, preferred_element_type=jnp.float32) * scale
        causal = key_pos[None, :] < q_pos[:, None]
        log_beta = jax.nn.log_sigmoid(z)
        log_1m_beta = jnp.where(causal, jax.nn.log_sigmoid(-z), 0.0)
        log_stay = lax.cumsum(log_1m_beta, axis=3, reverse=True) - log_1m_beta
        a = jnp.where(causal, jnp.exp(log_beta + log_stay), 0.0)
        o = jnp.einsum('bhqk,bhkd->bhqd', a.astype(v.dtype), v, preferred_element_type=jnp.float32)
        return o.astype(q.dtype)

    out = lax.map(block_fn, (jnp.arange(n_qblk), q_blocks))
    return jnp.moveaxis(out, 0, 2).reshape(B, H, S, Dh)


def setup_inputs(seed: int = 0) -> dict:
    key = jax.random.key(seed)
    ks = jax.random.split(key, 12)
    f32 = jnp.float32
    x = jax.random.normal(ks[0], (BATCH, SEQ, D_MODEL), f32)
    mix_norm_g = 1.0 + 0.02 * jax.random.normal(ks[1], (DEPTH, D_MODEL), f32)
    w_in = jax.random.normal(ks[2], (DEPTH, D_MODEL, D_QKV), f32) * D_MODEL ** -0.5
    moba_out_g = 1.0 + 0.02 * jax.random.normal(ks[3], (DEPTH, D_MOBA), f32)
    sb_out_g = 1.0 + 0.02 * jax.random.normal(ks[4], (DEPTH, D_SB), f32)
    w_out = jax.random.normal(ks[5], (DEPTH, D_MIX, D_MODEL), f32) * D_MIX ** -0.5
    mlp_norm_g = 1.0 + 0.02 * jax.random.normal(ks[6], (DEPTH, D_MODEL), f32)
    w_up = jax.random.normal(ks[7], (DEPTH, D_MODEL, D_FF), f32) * D_MODEL ** -0.5
    w_down = jax.random.normal(ks[8], (DEPTH, D_FF, D_MODEL), f32) * D_FF ** -0.5
    final_norm_g = 1.0 + 0.02 * jax.random.normal(ks[9], (D_MODEL,), f32)
    return {"x": x, "mix_norm_g": mix_norm_g, "w_in": w_in, "moba_out_g": moba_out_g,
            "sb_out_g": sb_out_g, "w_out": w_out, "mlp_norm_g": mlp_norm_g,
            "w_up": w_up, "w_down": w_down, "final_norm_g": final_norm_g}


def reference(x, mix_norm_g, w_in, moba_out_g, sb_out_g, w_out, mlp_norm_g, w_up, w_down,
              final_norm_g):
    B, S, _ = x.shape
    pos = jnp.arange(S)

    def heads(t, n_heads):
        return t.reshape(B, S, n_heads, HEAD_DIM).transpose(0, 2, 1, 3)

    def merge(t):
        return t.transpose(0, 2, 1, 3).reshape(B, S, -1)

    for l in range(DEPTH):
        h = rmsnorm(x, mix_norm_g[l])
        qkv = jnp.einsum('bsd,de->bse', h, w_in[l])
        q_a, k_a, v_a, q_b, k_b, v_b = jnp.split(
            qkv, [D_MOBA, 2 * D_MOBA, 3 * D_MOBA, 3 * D_MOBA + D_SB, 3 * D_MOBA + 2 * D_SB], axis=-1)
        q_a = partial_rope(heads(q_a, N_HEADS_MOBA), pos)
        k_a = partial_rope(heads(k_a, N_HEADS_MOBA), pos)
        o_a = moba_attention(q_a, k_a, heads(v_a, N_HEADS_MOBA))
        o_b = stick_breaking_attention(heads(q_b, N_HEADS_SB), heads(k_b, N_HEADS_SB),
                                       heads(v_b, N_HEADS_SB))
        o = jnp.concatenate([rmsnorm(merge(o_a), moba_out_g[l]),
                             rmsnorm(merge(o_b), sb_out_g[l])], axis=-1)
        x = x + jnp.einsum('bse,ed->bsd', o, w_out[l])
        h = rmsnorm(x, mlp_norm_g[l])
        u = jnp.einsum('bsd,df->bsf', h, w_up[l])
        x = x + jnp.einsum('bsf,fd->bsd', jnp.square(jax.nn.relu(u)), w_down[l])
    return rmsnorm(x, final_norm_g)
```

```python
from contextlib import ExitStack

import numpy as np
import ml_dtypes
import concourse.bass as bass
import concourse.mybir as mybir
from concourse.bass_utils import run_bass_kernel_spmd

F32 = mybir.dt.float32
BF16 = mybir.dt.bfloat16
AF = mybir.ActivationFunctionType
ALU = mybir.AluOpType
AX = mybir.AxisListType

D = 2048
S = 2048
HALF = 1024
NH = 16
EPS = 1e-6
SCALE = 128 ** -0.5
NEGBIG = 30000.0


class Prog:
    ENGS = ("pe", "act", "dve", "pool", "sp")

    def __init__(self):
        self.q = {e: [] for e in self.ENGS}
        self.cnt = {e: 0 for e in self.ENGS}
        self.known = {e: {} for e in self.ENGS}
        self.slots = []

    def slot(self, name):
        self.cnt[name] = 0
        self.slots.append(name)
        return name

    def op(self, eng, fn, deps=(), sig=True, slot=None):
        waits = []
        for d in self._flat(deps):
            key, val = d
            if key == "pe" and eng == "pe":
                continue
            if self.known[eng].get(key, 0) < val:
                waits.append((key, val))
                self.known[eng][key] = val
        if slot is not None:
            self.cnt[slot] += 16
            tok = (slot, self.cnt[slot])
            inc = (slot, 16)
        elif sig:
            self.cnt[eng] += 1
            tok = (eng, self.cnt[eng])
            inc = (eng, 1)
        else:
            tok = None
            inc = None
        self.q[eng].append((waits, fn, inc))
        return tok

    def _flat(self, deps):
        out = []
        if deps is None:
            return out
        if isinstance(deps, tuple) and len(deps) == 2 and isinstance(deps[0], str):
            return [deps]
        for d in deps:
            out.extend(self._flat(d))
        return out


class Ring:
    def __init__(self, bufs):
        self.bufs = bufs
        self.free = [[] for _ in bufs]
        self.i = 0

    def get(self):
        k = self.i % len(self.bufs)
        self.i += 1
        return k, self.bufs[k], self.free[k]

    def release(self, k, toks):
        self.free[k] = toks


def build_program(stage=99):
    nc = bass.Bass("TRN2", target_bir_lowering=False)
    P = Prog()

    def din(name, shape, dt=F32):
        return nc.dram_tensor(name, list(shape), dt, kind="ExternalInput").ap()

    xc = din("xc", [S, D])
    w_in_h = din("w_in_h", [NH, 128, 16 * 384])
    w_out_r = din("w_out_r", [4, 128, 16 * 512])
    w_up_r = din("w_up_r", [16, 128, 4 * 16 * 128])
    w_down = din("w_down", [8192, D])
    g1_bc = din("g1_bc", [128, D])
    g2_bc = din("g2_bc", [128, D])
    gf_bc = din("gf_bc", [128, D])
    go_col_d = din("go_col", [128, NH])
    ropeC_d = din("ropeC", [32, S])
    ropeS_d = din("ropeS", [32, S])
    perm_d = din("perm", [32, 128], BF16)
    mask_sb_d = din("mask_sb", [128, 4 * 512], BF16)
    mask_mb_d = din("mask_mb", [128, 4 * 512], BF16)
    gbias_d = din("gbias", [128, 64])
    ownb_d = din("ownb", [128, 64])
    ident_bf_d = din("ident_bf", [128, 128], BF16)
    tri_d = din("tri", [128, 128], BF16)
    negones_d = din("negones", [128, 128], BF16)
    ones_d = din("ones", [128, 128], BF16)
    onehot_d = din("onehot", [8, 8 * 128], BF16)
    out_d = nc.dram_tensor("out", [HALF, D], F32, kind="ExternalOutput").ap()

    es = ExitStack()
    with es:
        def sb(name, shape, dt):
            return es.enter_context(nc.sbuf_tensor(name, list(shape), dt))

        def ps(name, shape, dt):
            return es.enter_context(nc.psum_tensor(name, list(shape), dt))

        arenaA = sb("arenaA", [128, 32768], BF16)
        hT = arenaA[:].rearrange("p (c t) -> p c t", c=16)
        x1 = arenaA[:].bitcast(F32).rearrange("p (t e) -> p t e", t=8)
        arenaB = sb("arenaB", [128, 16384], BF16)
        onT = arenaB[:].rearrange("p (h t) -> p h t", h=16)
        h2T = onT
        arenaC = sb("arenaC", [128, 24576], BF16)
        cC = arenaC[:]
        wh = [cC[:, i * 6144:(i + 1) * 6144].rearrange("p (c n) -> p c n", c=16) for i in range(2)]
        ropeC = cC[0:32, 12288:16384].bitcast(F32)
        ropeS = cC[0:32, 16384:20480].bitcast(F32)
        mask_sb = cC[:, 20480:22528].rearrange("p (j q) -> p j q", j=4)
        mask_mb = cC[:, 22528:24576].rearrange("p (j q) -> p j q", j=4)
        wo = [cC[:, i * 8192:(i + 1) * 8192].rearrange("p (h e) -> p h e", h=16) for i in range(2)]
        wu = [cC[:, i * 8192:(i + 1) * 8192].rearrange("p (f c n) -> p f c n", f=4, c=16) for i in range(2)]
        wd1 = cC[:, 16384:24576].rearrange("p (f e) -> p f e", f=4)
        arenaD = sb("arenaD", [128, 20480], BF16)
        cD = arenaD[:]
        xt = [cD[:, i * 4096:(i + 1) * 4096].bitcast(F32) for i in range(2)]
        hn = [cD[:, 8192 + i * 2048:8192 + (i + 1) * 2048] for i in range(2)]
        junk = cD[:, 12288:14336]
        gbc = cD[:, 14336:18432].bitcast(F32)
        yo = xt
        QT = cD[:, 0:1024]
        KT = cD[:, 1024:3072]
        Vt = cD[:, 3072:5120].rearrange("p (t d) -> p t d", t=16)
        Ef = [cD[:, 5120 + i * 1024:5120 + (i + 1) * 1024].bitcast(F32) for i in range(2)]
        Lp = [cD[:, 7168 + i * 512:7168 + (i + 1) * 512] for i in range(2)]
        Ab = [cD[:, 8192 + i * 512:8192 + (i + 1) * 512] for i in range(2)]
        Rb = [cD[:, 9216 + i * 512:9216 + (i + 1) * 512] for i in range(2)]
        Of = cD[:, 10240:11264].bitcast(F32)
        rden = cD[:, 11264:12288].bitcast(F32)
        sqb = cD[:, 12288:12800]
        raw32 = cD[0:32, 12800:13824].bitcast(F32)
        ropeA = cD[0:32, 13824:14848].bitcast(F32)
        ropeB = cD[0:32, 14848:15872].bitcast(F32)
        selbT = cD[0:8, 15872:16896]
        rhi = cD[0:32, 16896:17408]
        rlo = cD[0:32, 17408:17920]
        ATs = [cD[:, i * 4096:(i + 1) * 4096].rearrange("p (f t) -> p f t", f=4) for i in range(2)]
        rls = [cD[:, 8192 + i * 1024:8192 + (i + 1) * 1024].bitcast(F32) for i in range(2)]

        go_col = sb("go_col_s", [128, NH], F32)
        perm = sb("perm_s", [32, 128], BF16)
        gbias = sb("gbias_s", [128, 64], F32)
        ownb = sb("ownb_s", [128, 64], F32)
        ident_bf = sb("ident_bf_s", [128, 128], BF16)
        tri = sb("tri_s", [128, 128], BF16)
        negones = sb("negones_s", [128, 128], BF16)
        ones = sb("ones_s", [128, 128], BF16)
        onehot = sb("onehot_s", [8, 8, 128], BF16)
        cbias = sb("cbias", [128, 4], F32)
        stat = sb("stat", [128, 256], F32)
        ssq1 = stat[:, 0:16]
        rs1 = stat[:, 16:32]
        rstd1 = stat[:, 32:48]
        ssqA = stat[:, 48:56]
        ssqB = stat[:, 56:64]
        rsAB = stat[:, 64:80]
        rstdAB = stat[:, 80:96]
        ssq2 = stat[:, 96:104]
        rs2 = stat[:, 104:112]
        rstd2 = stat[:, 112:120]
        ssq3 = stat[:, 120:128]
        rs3 = stat[:, 128:136]
        rstd3 = stat[:, 136:144]
        km = stat[:, 144:152]
        kms = stat[:, 152:160]
        gsm = sb("gsm", [128, 3, 64], F32)
        gm = gsm[:, 0, :]
        top = gsm[:, 1, :]
        sel = gsm[:, 2, :]
        selb = sb("selb", [128, 64], BF16)
        kmb = sb("kmb", [128, 16], BF16)
        km_hi = kmb[:, 0:8]
        km_lo = kmb[:, 8:16]

        ps_mm = [ps(f"ps_mm{i}", [128, 512], F32) for i in range(3)]
        ps_acc = [ps(f"ps_acc{i}", [128, 512], F32) for i in range(2)]
        ps_small = ps("ps_small", [128, 512], F32)
        ps_tp = [ps(f"ps_tp{i}", [128, 4, 128], BF16)[:] for i in range(2)]

        sems = {}
        for e in Prog.ENGS:
            sems[e] = es.enter_context(nc.semaphore("sem_" + e))

        def newslot(name):
            sems[name] = es.enter_context(nc.semaphore("sl_" + name))
            return P.slot(name)

        mm_ring = Ring(ps_mm)
        tp_ring = Ring(ps_tp)

        all_dma = {}

        def dma(q, out, in_, slot, deps=()):
            tok = P.op(q, lambda e, out=out, in_=in_: e.dma_start(out=out, in_=in_), deps, slot=slot)
            all_dma[slot] = tok
            return tok

        def mm(out, lhsT, rhs, start, stop, deps=(), sig=False):
            return P.op("pe", lambda e, out=out, lhsT=lhsT, rhs=rhs, start=start, stop=stop:
                        e.matmul(out, lhsT=lhsT, rhs=rhs, start=start, stop=stop), deps, sig=sig)

        def tpose(out, in_, ident, deps=(), sig=False):
            return P.op("pe", lambda e, out=out, in_=in_, ident=ident: e.transpose(out=out, in_=in_, identity=ident),
                        deps, sig=sig)

        def act(out, in_, func, deps=(), scale=1.0, bias=0.0, accum_out=None):
            def f(e, out=out, in_=in_, func=func, scale=scale, bias=bias, accum_out=accum_out):
                kw = {}
                if accum_out is not None:
                    kw["accum_out"] = accum_out
                np_ = in_.shape[0]
                if bias == 1.0:
                    kw["bias"] = cbias[0:np_, 1:2]
                elif bias == EPS:
                    kw["bias"] = cbias[0:np_, 2:3]
                return e.activation(out=out, in_=in_, func=func, scale=scale, **kw)
            return P.op("act", f, deps)

        def vcopy(eng, out, in_, deps=()):
            if eng == "act":
                return act(out, in_, AF.Copy, deps)
            return P.op(eng, lambda e, out=out, in_=in_: e.tensor_copy(out=out, in_=in_), deps)

        def tt(eng, out, in0, in1, op, deps=()):
            return P.op(eng, lambda e, out=out, in0=in0, in1=in1, op=op: e.tensor_tensor(out=out, in0=in0, in1=in1, op=op), deps)

        def ts(eng, out, in0, s1, op0, s2=None, op1=None, deps=()):
            def f(e, out=out, in0=in0, s1=s1, op0=op0, s2=s2, op1=op1):
                if op1 is None:
                    return e.tensor_scalar(out=out, in0=in0, scalar1=s1, scalar2=None, op0=op0)
                return e.tensor_scalar(out=out, in0=in0, scalar1=s1, scalar2=s2, op0=op0, op1=op1)
            return P.op(eng, f, deps)

        def stt(eng, out, in0, scalar, in1, op0, op1, deps=()):
            return P.op(eng, lambda e, out=out, in0=in0, scalar=scalar, in1=in1, op0=op0, op1=op1:
                        e.scalar_tensor_tensor(out=out, in0=in0, scalar=scalar, in1=in1, op0=op0, op1=op1), deps)

        class _Stop(Exception):
            pass

        dslot = []

        def dump(src_f32, deps):
            if not dslot:
                dslot.append(newslot("dump"))
            n = src_f32.shape[1]
            ov = out_d.rearrange("(p a) e -> p (a e)", p=128)
            st = None
            for c0 in range(0, n, 4096):
                c1 = min(n, c0 + 4096)
                st = dma("sp", ov[:, c0:c1], src_f32[:, c0:c1], dslot[0], [deps, list(all_dma.values())])
            P.op("sp", lambda e: e.nop(), [st], sig=False)
            raise _Stop()

        def plan():
            cs = newslot("consts")
            ctok = None
            for (dst, src) in [(go_col[:], go_col_d), (ropeC, ropeC_d), (ropeS, ropeS_d), (perm[:], perm_d),
                               (mask_sb, mask_sb_d.rearrange("p (j q) -> p j q", j=4)),
                               (mask_mb, mask_mb_d.rearrange("p (j q) -> p j q", j=4)),
                               (gbias[:], gbias_d), (ownb[:], ownb_d), (ident_bf[:], ident_bf_d),
                               (tri[:], tri_d), (negones[:], negones_d), (ones[:], ones_d),
                               (onehot[:], onehot_d.rearrange("p (n k) -> p n k", n=8))]:
                ctok = dma("sp", dst, src, cs)
            gslot = newslot("gbc")
            gtok = dma("sp", gbc[:], g1_bc, gslot)
            P.op("dve", lambda e: e.memset(stat[:], 0.0), sig=False)
            P.op("dve", lambda e: e.memset(cbias[:, 0:1], 0.0), sig=False)
            P.op("dve", lambda e: e.memset(cbias[:, 1:2], 1.0), sig=False)
            t0 = P.op("dve", lambda e: e.memset(cbias[:, 2:3], EPS))
            if stage == 0:
                dump(cC[:, 12288:24576].bitcast(F32), [ctok, gtok, t0])
            if stage == -1:
                dump(stat[:], [t0])

            xs = [newslot("xs0"), newslot("xs1")]
            xfree = [[], []]
            hnfree = [[], []]
            junk_tok = [None]

            def norm_transpose(src_tile_ap, ntok_tiles, ssq, rs, rstd, dstT, gt, load_fn, extra_deps=()):
                last = []
                for tti in range(ntok_tiles):
                    k = tti % 2
                    xin, ltok = load_fn(tti, k, xfree[k])
                    a1 = act(junk, xin, AF.Square, [ltok, junk_tok[0], t0, extra_deps], accum_out=ssq[:, tti:tti + 1])
                    junk_tok[0] = a1
                    if stage == 10:
                        dump(stat[:], [a1])
                    a2 = act(rs[:, tti:tti + 1], ssq[:, tti:tti + 1], AF.Sqrt, [a1], scale=1.0 / D, bias=EPS)
                    if stage == 11:
                        dump(stat[:], [a2])
                    v1 = P.op("dve", lambda e, o=rstd[:, tti:tti + 1], i=rs[:, tti:tti + 1]: e.reciprocal(out=o, in_=i), [a2])
                    v2 = stt("dve", hn[k], xin, rstd[:, tti:tti + 1], gbc[:], ALU.mult, ALU.mult, [v1, gt, hnfree[k], ltok])
                    if stage == 12:
                        dump(stat[:], [v1])
                    if stage == 13:
                        dump(cD[:, 8192:12288].bitcast(F32), [v2])
                    xfree[k] = [a1, v2]
                    tps = []
                    for g in range(4):
                        rk, pt, fr = tp_ring.get()
                        tl = None
                        for j in range(4):
                            c = g * 4 + j
                            tl = tpose(pt[:, j, :], hn[k][:, c * 128:(c + 1) * 128], ident_bf[:],
                                       [v2, fr, ctok] if j == 0 else (), sig=(j == 3))
                        eng = "act"
                        cp = vcopy(eng, dstT[:, g * 4:(g + 1) * 4, tti * 128:(tti + 1) * 128], pt, [tl, extra_deps])
                        if stage == 16 + g:
                            dump(stat[:], [cp])
                        tp_ring.release(rk, [cp])
                        tps.append(tl)
                        last.append(cp)
                    hnfree[k] = tps
                    if stage == 14:
                        dump(arenaA[:, 0:8192].bitcast(F32), last)
                return last

            def load_ctx(tti, k, free):
                tok = dma("sp", xt[k], xc[tti * 128:(tti + 1) * 128, :], xs[k], free)
                return xt[k], tok

            hT_ready = norm_transpose(None, 16, ssq1, rs1, rstd1, hT, gtok, load_ctx)
            p1_done = list(hT_ready)
            if stage == 1:
                dump(arenaA[:].bitcast(F32), p1_done)

            whfree = [[], []]
            wh_tok = {}

            def load_wh(h):
                k = head_list.index(h) % 2
                deps_ = whfree[k]
                sl_ = newslot(f"wh_{h}")
                wh_tok[h] = dma("pool", wh[k].rearrange("p c n -> p (c n)"), w_in_h[h], sl_, deps_)

            head_list = list(range(NH))
            if stage == 5:
                head_list = [8]
            for h_ in head_list[:2]:
                load_wh(h_)
            qkv_free = list(p1_done)
            acc_free = [[], []]
            small_free = []
            onT_written = []
            ssq_tok = {0: t0, 1: t0}
            evac_i = [0]
            last_proj_mm = None

            for hi_, h in enumerate(head_list):
                moba = h < 8
                k = hi_ % 2
                W = wh[k]
                wtok = wh_tok[h]
                head_reads = []
                proj_evac = []
                for which, ntile, dst, col0, sc in (("q", 2, QT, 0, SCALE), ("k", 4, KT, 128, 1.0)):
                    for ti in range(ntile):
                        pos0 = (HALF + ti * 512) if which == "q" else ti * 512
                        rk, pm, fr = mm_ring.get()
                        for c in range(16):
                            lt = mm(pm[:], W[:, c, col0:col0 + 128], hT[:, c, pos0:pos0 + 512], c == 0, c == 15,
                                    [wtok, fr, hT_ready] if c == 0 else (), sig=(c == 15))
                        dsl = dst[:, ti * 512:(ti + 1) * 512]
                        if moba:
                            e1 = act(raw32, pm[0:32, :], AF.Copy, [lt, qkv_free], scale=sc)
                            if stage == 20:
                                dump(stat[:], [e1])
                            h1 = vcopy("dve", rhi, raw32, [e1, qkv_free])
                            h2 = tt("dve", rlo, raw32, rhi, ALU.subtract, [h1])
                            rk2, pm2, fr2 = mm_ring.get()
                            mm(pm2[:], perm[:], rhi, True, False, [h1, fr2, ctok])
                            sw = mm(pm2[:], perm[:], rlo, False, True, [h2], sig=True)
                            d1 = tt("dve", ropeA, raw32, ropeC[:, pos0:pos0 + 512], ALU.mult, [e1, ctok])
                            d2 = tt("dve", ropeB, pm2[0:32, :], ropeS[:, pos0:pos0 + 512], ALU.mult, [sw, ctok])
                            if stage == 21:
                                dump(stat[:], [d1, d2])
                            e2 = act(dsl, pm[:], AF.Copy, [lt, qkv_free], scale=sc)
                            d3 = tt("dve", dsl[0:32, :], ropeA, ropeB, ALU.add, [d1, d2, e2, qkv_free])
                            mm_ring.release(rk2, [d2])
                            mm_ring.release(rk, [e1, e2])
                            proj_evac += [d3, e2]
                            qkv_free = [qkv_free, d3]
                        else:
                            evac_i[0] += 1
                            eng = "act" if evac_i[0] % 2 == 0 else "dve"
                            if eng == "act":
                                e2 = act(dsl, pm[:], AF.Copy, [lt, qkv_free], scale=sc)
                            else:
                                e2 = ts("dve", dsl, pm[:], sc, ALU.mult, deps=[lt, qkv_free])
                            mm_ring.release(rk, [e2])
                            proj_evac.append(e2)
                if stage == 23:
                    dump(cD[:, 0:5120].bitcast(F32), [proj_evac])
                for g in range(4):
                    rk, pm, fr = mm_ring.get()
                    for j in range(4):
                        tti = g * 4 + j
                        for c in range(16):
                            lt = mm(pm[:, j * 128:(j + 1) * 128], hT[:, c, tti * 128:(tti + 1) * 128], W[:, c, 256:384],
                                    c == 0, c == 15, [wtok, fr, hT_ready] if (j == 0 and c == 0) else (),
                                    sig=(j == 3 and c == 15))
                    evac_i[0] += 1
                    eng = "act" if evac_i[0] % 2 == 0 else "dve"
                    if stage == 24:
                        eng = "act"
                    if stage == 25:
                        eng = "dve"
                    e2 = vcopy(eng, Vt[:, g * 4:(g + 1) * 4, :], pm[:].rearrange("p (t d) -> p t d", t=4), [lt, qkv_free])
                    if stage in (24, 25) and g == 1:
                        dump(cD[:, 0:5120].bitcast(F32), [proj_evac, e2])
                    if stage == 26 and g == 2:
                        dump(cD[:, 0:5120].bitcast(F32), [proj_evac, e2])
                    if stage == 27 and g == 3:
                        dump(cD[:, 0:5120].bitcast(F32), [proj_evac, e2])
                    mm_ring.release(rk, [e2])
                    proj_evac.append(e2)
                last_proj_mm = lt
                whfree[k] = [lt]
                if stage in (2, 28, 29) and h == 0:
                    dump(cD[:, 0:5120].bitcast(F32), [proj_evac])
                if hi_ + 2 < len(head_list):
                    load_wh(head_list[hi_ + 2])

                if moba:
                    r1 = P.op("dve", lambda e: e.tensor_reduce(out=km, in_=KT.rearrange("p (n k) -> p n k", n=8),
                                                               axis=AX.X, op=ALU.add), [proj_evac])
                    r2 = ts("dve", kms, km, 1.0 / 256.0, ALU.mult, deps=[r1])
                    r3 = vcopy("dve", km_hi, kms, [r2])
                    r4 = tt("dve", km_lo, kms, km_hi, ALU.subtract, [r3])
                    for j in range(8):
                        mm(ps_small[:, j * 8:(j + 1) * 8], QT[:, j * 128:(j + 1) * 128], km_hi, True, False,
                           [r4, proj_evac, small_free] if j == 0 else ())
                        gl = mm(ps_small[:, j * 8:(j + 1) * 8], QT[:, j * 128:(j + 1) * 128], km_lo, False, True, sig=(j == 7))
                    g1 = tt("dve", gm, ps_small[:, 0:64], gbias[:], ALU.add, [gl, ctok])
                    small_free = [g1]
                    gx = g1
                    for j in range(8):
                        gx = P.op("dve", lambda e, o=top[:, j * 8:(j + 1) * 8], i=gm[:, j * 8:(j + 1) * 8]: e.max(out=o, in_=i), [gx])
                    for j in range(8):
                        gx = ts("dve", sel[:, j * 8:(j + 1) * 8], gm[:, j * 8:(j + 1) * 8], top[:, j * 8 + 3:j * 8 + 4],
                                ALU.is_gt, deps=[gx])
                    gx = tt("dve", sel, sel, ownb[:], ALU.add, [gx])
                    gx = ts("dve", selb[:], sel, -1.0, ALU.add, NEGBIG, ALU.mult, deps=[gx])
                    sel_cp = []
                    for half_ in range(2):
                        rk, pt, fr = tp_ring.get()
                        for j in range(4):
                            jj = half_ * 4 + j
                            tl = tpose(pt[0:8, j, :], selb[:, jj * 8:(jj + 1) * 8], ident_bf[:],
                                       [gx, fr, ctok] if j == 0 else (), sig=(j == 3))
                        cp = act(selbT[:, half_ * 512:(half_ + 1) * 512], pt[0:8].rearrange("p j t -> p (j t)"), AF.Copy,
                                 [tl, qkv_free])
                        tp_ring.release(rk, [cp])
                        sel_cp.append(cp)

                if stage == 3 and h == 0:
                    dump(gsm[:].rearrange("p a b -> p (a b)"), [gx, sel_cp])
                for Qi in range(2):
                    qs = QT[:, Qi * 512:(Qi + 1) * 512]
                    last_kt = 8 + 4 * Qi + 3
                    accO = ps_acc[0]
                    accD = ps_acc[1]
                    if moba:
                        kts = list(range(0, last_kt + 1))
                    else:
                        kts = list(range(last_kt, -1, -1))
                    pend = None
                    rcur = None
                    for idx, kt in enumerate(kts):
                        first = idx == 0
                        lastk = idx == len(kts) - 1
                        diag = kt >= 8 + 4 * Qi
                        jd = kt - (8 + 4 * Qi)
                        r = idx % 2
                        ks = KT[:, kt * 128:(kt + 1) * 128]
                        if moba:
                            rk, pm, fr = mm_ring.get()
                            mm(pm[:], ks, qs, True, False, [proj_evac, fr, sel_cp])
                            s1 = mm(pm[:], onehot[:, kt // 2, :], selbT[:, Qi * 512:(Qi + 1) * 512], False, True, sig=True)
                            e1 = act(Ab[r], pm[:], AF.Exp, [s1, head_reads])
                            mm_ring.release(rk, [e1])
                            pt = e1
                            if diag:
                                pt = tt("dve", Ab[r], Ab[r], mask_mb[:, jd, :], ALU.mult, [e1, ctok])
                            mm(accO[:], Vt[:, kt, :], Ab[r], first, lastk, [pt, acc_free[0]] if first else [pt])
                            o2 = mm(accD[:], ones[:], Ab[r], first, lastk, [acc_free[1]] if first else (), sig=True)
                            head_reads = [o2]
                            fin = o2
                        else:
                            rk, pm, fr = mm_ring.get()
                            z1 = mm(pm[:], ks, qs, True, True, [proj_evac, fr], sig=True)
                            e1 = act(Ef[r], pm[:], AF.Exp, [z1, head_reads])
                            mm_ring.release(rk, [e1])
                            e2 = act(Lp[r], Ef[r], AF.Ln, [e1], bias=1.0)
                            lt_ = e2
                            if diag:
                                lt_ = tt("dve", Lp[r], Lp[r], mask_sb[:, jd, :], ALU.mult, [e2, ctok])
                            rk2, pg, fr2 = mm_ring.get()
                            mm(pg[:], ks, qs, True, False, [fr2])
                            g2 = mm(pg[:], tri[:], Lp[r], False, first, [lt_, ctok], sig=first)
                            if not first:
                                g2 = mm(pg[:], negones[:], Rb[rcur], False, True, [rtok], sig=True)
                            e3 = act(Ab[r], pg[:], AF.Exp, [g2, head_reads])
                            mm_ring.release(rk2, [e3])
                            at_ = e3
                            if diag:
                                at_ = tt("dve", Ab[r], Ab[r], mask_sb[:, jd, :], ALU.mult, [e3])
                            o2 = mm(accO[:], Vt[:, kt, :], Ab[r], first, lastk, [at_, acc_free[0]] if first else [at_], sig=True)
                            if not lastk:
                                if first:
                                    rtok = vcopy("dve", Rb[0], Lp[r], [lt_, head_reads])
                                    rcur = 0
                                else:
                                    rtok = tt("dve", Rb[1 - rcur], Rb[rcur], Lp[r], ALU.add, [lt_, rtok, g2, head_reads])
                                    rcur = 1 - rcur
                            head_reads = [o2, g2]
                            fin = o2
                    if moba:
                        v1 = P.op("dve", lambda e: e.reciprocal(out=rden, in_=accD[:]), [fin, onT_written[-2:]])
                        v2 = tt("dve", Of, accO[:], rden, ALU.mult, [v1])
                        acc_free = [[v2], [v1]]
                    else:
                        v2 = vcopy("dve", Of, accO[:], [fin, onT_written[-2:]])
                        acc_free = [[v2], acc_free[1]]
                    a1 = act(onT[:, h, Qi * 512:(Qi + 1) * 512], Of, AF.Copy, [v2, ctok], scale=go_col[:, h:h + 1])
                    a2 = act(sqb, Of, AF.Square, [v2, onT_written[-2:]])
                    for j in range(4):
                        sl = mm(ps_small[:, 64 + j:65 + j], sqb[:, j * 128:(j + 1) * 128], ones[:, 0:1], True, True,
                                [a2, small_free] if j == 0 else (), sig=(j == 3))
                    gi = 0 if moba else 1
                    sacc = ssqA if moba else ssqB
                    v3 = tt("dve", sacc[:, Qi * 4:(Qi + 1) * 4], sacc[:, Qi * 4:(Qi + 1) * 4], ps_small[:, 64:68], ALU.add,
                            [sl, ssq_tok[gi]])
                    ssq_tok[gi] = v3
                    small_free = [small_free, v3]
                    onT_written += [a1, a2, sl]
                    head_reads = [head_reads, a1, a2]
                qkv_free = [head_reads, onT_written[-6:]]
                if (stage == 4 and h == 0) or (stage == 5 and h == 8):
                    dump(arenaB[:, h * 1024:(h + 1) * 1024].bitcast(F32), [onT_written, qkv_free, ssq_tok[0], ssq_tok[1]])

            att_done = [onT_written, ssq_tok[0], ssq_tok[1], qkv_free]

            x1s = newslot("x1ld")
            x1tok = None
            for tti in range(8):
                x1tok = dma("sp", x1[:, tti, :], xc[HALF + tti * 128:HALF + (tti + 1) * 128, :], x1s, [last_proj_mm, att_done])
            g2tok = dma("sp", gbc[:], g2_bc, gslot, [p1_done, att_done])
            a_ = act(rsAB, stat[:, 48:64], AF.Sqrt, [att_done], scale=1.0 / 1024.0, bias=EPS)
            rAB = P.op("dve", lambda e: e.reciprocal(out=rstdAB, in_=rsAB), [a_])
            wofree = [[att_done], [att_done]]
            wo_tok = {}

            def load_wo(et):
                kk = et % 2
                sl_ = newslot(f"wo_{et}")
                for hh in range(2):
                    wo_tok[et] = dma("pool", wo[kk][:, hh * 8:(hh + 1) * 8, :].rearrange("p h e -> p (h e)"),
                                     w_out_r[et][:, hh * 4096:(hh + 1) * 4096], sl_, wofree[kk])

            load_wo(0)
            load_wo(1)
            x1_last = {}
            for et in range(4):
                kk = et % 2
                for tti in range(8):
                    toks = []
                    for gi in range(2):
                        rk, pm, fr = mm_ring.get()
                        for hh in range(8):
                            h = gi * 8 + hh
                            lt = mm(pm[:], onT[:, h, tti * 128:(tti + 1) * 128], wo[kk][:, h, :], hh == 0, hh == 7,
                                    [wo_tok[et], fr, att_done] if hh == 0 else (), sig=(hh == 7))
                        xsl = x1[:, tti, et * 512:(et + 1) * 512]
                        v = stt("dve", xsl, pm[:], rstdAB[:, gi * 8 + tti:gi * 8 + tti + 1], xsl, ALU.mult, ALU.add,
                                [lt, rAB, x1tok, x1_last.get((tti, et))])
                        x1_last[(tti, et)] = v
                        mm_ring.release(rk, [v])
                        toks.append(lt)
                wofree[kk] = [lt]
                if et + 2 < 4:
                    load_wo(et + 2)
            op_done = [x1_last[kk_] for kk_ in x1_last] + [lt]
            if stage == 6:
                dump(arenaA[:].bitcast(F32), op_done)

            def load_x1(tti, k, free):
                return x1[:, tti, :], None

            h2_ready = norm_transpose(None, 8, ssq2, rs2, rstd2, h2T, g2tok, load_x1, extra_deps=op_done)

            if stage == 7:
                dump(arenaB[:].bitcast(F32), h2_ready)
            wufree = [[op_done], [op_done]]
            wdfree = [[op_done]]
            wu_tok = {}
            wd_tok = {}

            def load_wu(fb):
                kk = fb % 2
                sl_ = newslot(f"wu_{fb}")
                for fc in range(4):
                    wu_tok[fb] = dma("pool", wu[kk][:, fc].rearrange("p c n -> p (c n)"),
                                     w_up_r[fb][:, fc * 2048:(fc + 1) * 2048], sl_, wufree[kk])

            def load_wd(fb):
                wd_tok[fb] = dma("pool", wd1, w_down[fb * 512:(fb + 1) * 512, :].rearrange("(c p) e -> p c e", p=128),
                                 newslot(f"wd_{fb}"), wdfree[0])

            load_wu(0)
            load_wd(0)
            load_wu(1)
            ATfree = [[], []]
            rlfree = [[], []]
            rli = 0
            for fb in range(16):
                kk = fb % 2
                at_w = []
                for fc in range(4):
                    for tq in range(2):
                        rk, pm, fr = mm_ring.get()
                        for c in range(16):
                            lt = mm(pm[:], wu[kk][:, fc, c, :], h2T[:, c, tq * 512:(tq + 1) * 512], c == 0, c == 15,
                                    [wu_tok[fb], fr, h2_ready] if c == 0 else (), sig=(c == 15))
                        ri = rli % 2
                        rli += 1
                        e1 = act(rls[ri][:], pm[:], AF.Relu, [lt, rlfree[ri]])
                        mm_ring.release(rk, [e1])
                        v = tt("dve", ATs[kk][:, fc, tq * 512:(tq + 1) * 512], rls[ri][:], rls[ri][:], ALU.mult, [e1, ATfree[kk]])
                        rlfree[ri] = [v]
                        at_w.append(v)
                wufree[kk] = [lt]
                if fb + 2 < 16:
                    load_wu(fb + 2)
                for tti in range(8):
                    for et in range(4):
                        rk, pm, fr = mm_ring.get()
                        for fc in range(4):
                            lt = mm(pm[:], ATs[kk][:, fc, tti * 128:(tti + 1) * 128], wd1[:, fc, et * 512:(et + 1) * 512],
                                    fc == 0, fc == 3, [wd_tok[fb], fr, at_w] if fc == 0 else (), sig=(fc == 3))
                        xsl = x1[:, tti, et * 512:(et + 1) * 512]
                        v = tt("dve", xsl, xsl, pm[:], ALU.add, [lt, x1_last.get((tti, et)), h2_ready])
                        x1_last[(tti, et)] = v
                        mm_ring.release(rk, [v])
                wdfree[0] = [lt]
                ATfree[kk] = [lt]
                if fb + 1 < 16:
                    load_wd(fb + 1)

            mlp_done = [x1_last[kk_] for kk_ in x1_last]
            gftok = dma("sp", gbc[:], gf_bc, gslot, [h2_ready])
            osl = [newslot("os0"), newslot("os1")]
            ofree = [[mlp_done], [mlp_done]]
            store_toks = []
            for tti in range(8):
                k = tti % 2
                a1 = act(junk, x1[:, tti, :], AF.Square, [mlp_done, junk_tok[0]], accum_out=ssq3[:, tti:tti + 1])
                junk_tok[0] = a1
                a2 = act(rs3[:, tti:tti + 1], ssq3[:, tti:tti + 1], AF.Sqrt, [a1], scale=1.0 / D, bias=EPS)
                v1 = P.op("dve", lambda e, o=rstd3[:, tti:tti + 1], i=rs3[:, tti:tti + 1]: e.reciprocal(out=o, in_=i), [a2])
                v2 = stt("dve", yo[k], x1[:, tti, :], rstd3[:, tti:tti + 1], gbc[:], ALU.mult, ALU.mult, [v1, gftok, ofree[k]])
                st = dma("sp", out_d[tti * 128:(tti + 1) * 128, :], yo[k], osl[k], [v2])
                ofree[k] = [st]
                store_toks.append(st)
            P.op("sp", lambda e: e.nop(), store_toks, sig=False)

        try:
            plan()
        except _Stop:
            pass

        engmap = {"pe": "tensor", "act": "scalar", "dve": "vector", "pool": "gpsimd", "sp": "sync"}
        with nc.Block() as block:
            for ename in Prog.ENGS:
                def body(e, ename=ename):
                    for (waits, fn, inc) in P.q[ename]:
                        for (key, val) in waits:
                            e.wait_ge(sems[key], val)
                        ins = fn(e)
                        if inc is not None:
                            ins.then_inc(sems[inc[0]], inc[1])
                getattr(block, engmap[ename])(body)
    return nc


_NC_CACHE = {}


def _bf(a):
    return np.ascontiguousarray(a.astype(ml_dtypes.bfloat16))


def make_in_maps(x, mix_norm_g, w_in, moba_out_g, sb_out_g, w_out, mlp_norm_g, w_up, w_down, final_norm_g):
    x = np.asarray(x, dtype=np.float32)
    w_in = np.asarray(w_in, dtype=np.float32)[0]
    w_out = np.asarray(w_out, dtype=np.float32)[0]
    w_up = np.asarray(w_up, dtype=np.float32)[0]
    w_down_ = np.ascontiguousarray(np.asarray(w_down, dtype=np.float32)[0])
    B = x.shape[0]

    heads = []
    for h in range(NH):
        if h < 8:
            qc, kc, vc = h * 128, 1024 + h * 128, 2048 + h * 128
        else:
            hh = h - 8
            qc, kc, vc = 3072 + hh * 128, 4096 + hh * 128, 5120 + hh * 128
        sl = np.concatenate([w_in[:, qc:qc + 128], w_in[:, kc:kc + 128], w_in[:, vc:vc + 128]], axis=1)
        heads.append(sl.reshape(16, 128, 384).transpose(1, 0, 2).reshape(128, 16 * 384))
    w_in_h = np.ascontiguousarray(np.stack(heads, 0))
    w_out_r = np.ascontiguousarray(
        w_out.reshape(16, 128, 4, 512).transpose(2, 1, 0, 3).reshape(4, 128, 16 * 512))
    w_up_r = np.ascontiguousarray(
        w_up.reshape(16, 128, 16, 4, 128).transpose(2, 1, 3, 0, 4).reshape(16, 128, 4 * 16 * 128))

    def bc(v):
        return np.ascontiguousarray(np.broadcast_to(np.asarray(v, np.float32).reshape(1, D), (128, D)))

    g1 = bc(np.asarray(mix_norm_g)[0])
    g2 = bc(np.asarray(mlp_norm_g)[0])
    gf = bc(np.asarray(final_norm_g))
    go = np.concatenate([np.asarray(moba_out_g, np.float32)[0], np.asarray(sb_out_g, np.float32)[0]])
    go_col = np.ascontiguousarray(go.reshape(16, 128).T)

    kk = np.arange(128)[:, None]
    qq = np.arange(512)[None, :]
    msb = np.stack([((128 * j + kk) < qq) for j in range(4)], 1).astype(np.float32)
    mmb = np.stack([((128 * j + kk) <= qq) for j in range(4)], 1).astype(np.float32)
    mask_sb = _bf(msb.reshape(128, 2048))
    mask_mb = _bf(mmb.reshape(128, 2048))
    perm = np.zeros((32, 128), np.float32)
    for m in range(32):
        perm[(m + 16) % 32, m] = 1.0
    ident = np.eye(128, dtype=np.float32)
    tri = np.where(np.arange(128)[:, None] >= np.arange(128)[None, :], -1.0, 0.0).astype(np.float32)
    onehot = np.zeros((8, 8, 128), np.float32)
    for n in range(8):
        onehot[n, n, :] = 1.0
    inv_freq = (np.float32(500000.0) ** (-np.arange(16, dtype=np.float32) / np.float32(16))).astype(np.float32)

    in_maps = []
    for c in range(2 * B):
        b, half = c // 2, c % 2
        if half == 1:
            xcx = np.ascontiguousarray(x[b])
            pos = np.arange(S, dtype=np.float32)
        else:
            xcx = np.concatenate([np.zeros((HALF, D), np.float32), x[b, :HALF]], 0)
            pos = np.maximum(np.arange(S, dtype=np.float32) - HALF, 0).astype(np.float32)
        ang = pos[None, :] * inv_freq[:, None]
        cos = np.cos(ang).astype(np.float32)
        sin = np.sin(ang).astype(np.float32)
        ropeC = np.concatenate([cos, cos], 0)
        ropeS = np.concatenate([-sin, sin], 0)
        gb = np.zeros((128, 8, 8), np.float32)
        ob = np.zeros((128, 8, 8), np.float32)
        for j in range(8):
            qblk = 4 + j // 2
            for n in range(8):
                elig = (n < qblk) and (half == 1 or n >= 4)
                gb[:, j, n] = 0.0 if elig else -1e30
            ob[:, j, qblk] = 1.0
        in_maps.append({
            "xc": np.ascontiguousarray(xcx), "w_in_h": w_in_h, "w_out_r": w_out_r, "w_up_r": w_up_r, "w_down": w_down_,
            "g1_bc": g1, "g2_bc": g2, "gf_bc": gf, "go_col": go_col,
            "ropeC": np.ascontiguousarray(ropeC), "ropeS": np.ascontiguousarray(ropeS), "perm": _bf(perm),
            "mask_sb": mask_sb, "mask_mb": mask_mb,
            "gbias": np.ascontiguousarray(gb.reshape(128, 64)), "ownb": np.ascontiguousarray(ob.reshape(128, 64)),
            "ident_bf": _bf(ident), "tri": _bf(tri), "negones": _bf(-np.ones((128, 128), np.float32)),
            "ones": _bf(np.ones((128, 128), np.float32)), "onehot": _bf(onehot.reshape(8, 1024)),
        })

    return in_maps


def kernel(x, mix_norm_g, w_in, moba_out_g, sb_out_g, w_out, mlp_norm_g, w_up, w_down, final_norm_g):
    x = np.asarray(x, dtype=np.float32)
    B = x.shape[0]
    in_maps = make_in_maps(x, mix_norm_g, w_in, moba_out_g, sb_out_g, w_out, mlp_norm_g, w_up, w_down, final_norm_g)
    if "nc" not in _NC_CACHE:
        _NC_CACHE["nc"] = build_program()
    nc = _NC_CACHE["nc"]
    res = run_bass_kernel_spmd(nc, in_maps, core_ids=list(range(2 * B)))
    out = np.zeros((B, S, D), np.float32)
    for c in range(2 * B):
        b, half = c // 2, c % 2
        out[b, half * HALF:(half + 1) * HALF] = np.asarray(res.results[c]["out"], dtype=np.float32)
    return out
```

```python
from contextlib import ExitStack

import numpy as np
import ml_dtypes
import concourse.bass as bass
import concourse.mybir as mybir
from concourse.bass_utils import run_bass_kernel_spmd

F32 = mybir.dt.float32
BF16 = mybir.dt.bfloat16
AF = mybir.ActivationFunctionType
ALU = mybir.AluOpType
AX = mybir.AxisListType

D = 2048
S = 2048
HALF = 1024
NH = 16
EPS = 1e-6
SCALE = 128 ** -0.5
NEGBIG = 30000.0


class Prog:
    ENGS = ("pe", "act", "dve", "pool", "sp")

    def __init__(self):
        self.q = {e: [] for e in self.ENGS}
        self.cnt = {e: 0 for e in self.ENGS}
        self.known = {e: {} for e in self.ENGS}
        self.slots = []

    def slot(self, name):
        self.cnt[name] = 0
        self.slots.append(name)
        return name

    def op(self, eng, fn, deps=(), sig=True, slot=None):
        waits = []
        for d in self._flat(deps):
            key, val = d
            if key == "pe" and eng == "pe":
                continue
            if self.known[eng].get(key, 0) < val:
                waits.append((key, val))
                self.known[eng][key] = val
        if slot is not None:
            self.cnt[slot] += 16
            tok = (slot, self.cnt[slot])
            inc = (slot, 16)
        elif sig:
            self.cnt[eng] += 1
            tok = (eng, self.cnt[eng])
            inc = (eng, 1)
        else:
            tok = None
            inc = None
        self.q[eng].append((waits, fn, inc))
        return tok

    def _flat(self, deps):
        out = []
        if deps is None:
            return out
        if isinstance(deps, tuple) and len(deps) == 2 and isinstance(deps[0], str):
            return [deps]
        for d in deps:
            out.extend(self._flat(d))
        return out


class Ring:
    def __init__(self, bufs):
        self.bufs = bufs
        self.free = [[] for _ in bufs]
        self.i = 0

    def get(self):
        k = self.i % len(self.bufs)
        self.i += 1
        return k, self.bufs[k], self.free[k]

    def release(self, k, toks):
        self.free[k] = toks


def build_program(stage=99):
    nc = bass.Bass("TRN2", target_bir_lowering=False)
    P = Prog()

    def din(name, shape, dt=F32):
        return nc.dram_tensor(name, list(shape), dt, kind="ExternalInput").ap()

    xc = din("xc", [S, D])
    w_in_h = din("w_in_h", [NH, 128, 16 * 384])
    w_out_r = din("w_out_r", [4, 128, 16 * 512])
    w_up_r = din("w_up_r", [16, 128, 4 * 16 * 128])
    w_down = din("w_down", [8192, D])
    g1_bc = din("g1_bc", [128, D])
    g2_bc = din("g2_bc", [128, D])
    gf_bc = din("gf_bc", [128, D])
    go_col_d = din("go_col", [128, NH])
    ropeC_d = din("ropeC", [32, S])
    ropeS_d = din("ropeS", [32, S])
    perm_d = din("perm", [32, 128], BF16)
    mask_sb_d = din("mask_sb", [128, 4 * 512], BF16)
    mask_mb_d = din("mask_mb", [128, 4 * 512], BF16)
    gbias_d = din("gbias", [128, 64])
    ownb_d = din("ownb", [128, 64])
    ident_bf_d = din("ident_bf", [128, 128], BF16)
    tri_d = din("tri", [128, 128], BF16)
    negones_d = din("negones", [128, 128], BF16)
    ones_d = din("ones", [128, 128], BF16)
    onehot_d = din("onehot", [128, 8 * 128], BF16)
    out_d = nc.dram_tensor("out", [HALF, D], F32, kind="ExternalOutput").ap()

    es = ExitStack()
    with es:
        def sb(name, shape, dt):
            return es.enter_context(nc.sbuf_tensor(name, list(shape), dt))

        def ps(name, shape, dt):
            return es.enter_context(nc.psum_tensor(name, list(shape), dt))

        arenaA = sb("arenaA", [128, 32768], BF16)
        hT = arenaA[:].rearrange("p (c t) -> p c t", c=16)
        x1 = arenaA[:].bitcast(F32).rearrange("p (t e) -> p t e", t=8)
        arenaB = sb("arenaB", [128, 16384], BF16)
        onT = arenaB[:].rearrange("p (h t) -> p h t", h=16)
        h2T = onT
        arenaC = sb("arenaC", [128, 24576], BF16)
        cC = arenaC[:]
        wh = [cC[:, i * 6144:(i + 1) * 6144].rearrange("p (c n) -> p c n", c=16) for i in range(2)]
        ropeC = cC[0:32, 12288:16384].bitcast(F32)
        ropeS = cC[0:32, 16384:20480].bitcast(F32)
        mask_sb = cC[:, 20480:22528].rearrange("p (j q) -> p j q", j=4)
        mask_mb = cC[:, 22528:24576].rearrange("p (j q) -> p j q", j=4)
        wo = [cC[:, i * 8192:(i + 1) * 8192].rearrange("p (h e) -> p h e", h=16) for i in range(2)]
        wu = [cC[:, i * 8192:(i + 1) * 8192].rearrange("p (f c n) -> p f c n", f=4, c=16) for i in range(2)]
        wd1 = cC[:, 16384:24576].rearrange("p (f e) -> p f e", f=4)
        arenaD = sb("arenaD", [128, 20480], BF16)
        cD = arenaD[:]
        xt = [cD[:, i * 4096:(i + 1) * 4096].bitcast(F32) for i in range(2)]
        hn = [cD[:, 8192 + i * 2048:8192 + (i + 1) * 2048] for i in range(2)]
        junk = cD[:, 12288:14336]
        gbc = cD[:, 14336:18432].bitcast(F32)
        yo = xt
        QT = cD[:, 0:1024]
        KT = cD[:, 1024:3072]
        Vt = cD[:, 3072:5120].rearrange("p (t d) -> p t d", t=16)
        Ef = [cD[:, 5120 + i * 1024:5120 + (i + 1) * 1024].bitcast(F32) for i in range(2)]
        Lp = [cD[:, 7168 + i * 512:7168 + (i + 1) * 512] for i in range(2)]
        Ab = [cD[:, 8192 + i * 512:8192 + (i + 1) * 512] for i in range(2)]
        Rb = [cD[:, 9216 + i * 512:9216 + (i + 1) * 512] for i in range(2)]
        Of = cD[:, 10240:11264].bitcast(F32)
        rden = cD[:, 11264:12288].bitcast(F32)
        sqb = cD[:, 12288:12800]
        raw32 = cD[0:32, 12800:13824].bitcast(F32)
        ropeA = cD[0:32, 13824:14848].bitcast(F32)
        ropeB = cD[0:32, 14848:15872].bitcast(F32)
        selbT = cD[:, 17920:18944]
        rhi = cD[0:32, 16896:17408]
        rlo = cD[0:32, 17408:17920]
        ATs = [cD[:, i * 4096:(i + 1) * 4096].rearrange("p (f t) -> p f t", f=4) for i in range(2)]
        rls = [cD[:, 8192 + i * 1024:8192 + (i + 1) * 1024].bitcast(F32) for i in range(2)]

        go_col = sb("go_col_s", [128, NH], F32)
        perm = sb("perm_s", [32, 128], BF16)
        gbias = sb("gbias_s", [128, 64], F32)
        ownb = sb("ownb_s", [128, 64], F32)
        ident_bf = sb("ident_bf_s", [128, 128], BF16)
        tri = sb("tri_s", [128, 128], BF16)
        negones = sb("negones_s", [128, 128], BF16)
        ones = sb("ones_s", [128, 128], BF16)
        onehot = sb("onehot_s", [128, 8, 128], BF16)
        cbias = sb("cbias", [128, 4], F32)
        stat = sb("stat", [128, 256], F32)
        ssq1 = stat[:, 0:16]
        rs1 = stat[:, 16:32]
        rstd1 = stat[:, 32:48]
        ssqA = stat[:, 48:56]
        ssqB = stat[:, 56:64]
        rsAB = stat[:, 64:80]
        rstdAB = stat[:, 80:96]
        ssq2 = stat[:, 96:104]
        rs2 = stat[:, 104:112]
        rstd2 = stat[:, 112:120]
        ssq3 = stat[:, 120:128]
        rs3 = stat[:, 128:136]
        rstd3 = stat[:, 136:144]
        km = stat[:, 144:152]
        kms = stat[:, 152:160]
        gsm = sb("gsm", [128, 3, 64], F32)
        gm = gsm[:, 0, :]
        top = gsm[:, 1, :]
        sel = gsm[:, 2, :]
        selb = sb("selb", [128, 64], BF16)
        kmb = sb("kmb", [128, 16], BF16)
        km_hi = kmb[:, 0:8]
        km_lo = kmb[:, 8:16]

        ps_mm = [ps(f"ps_mm{i}", [128, 512], F32) for i in range(3)]
        ps_acc = [ps(f"ps_acc{i}", [128, 512], F32) for i in range(2)]
        ps_small = ps("ps_small", [128, 512], F32)
        ps_tp = [ps(f"ps_tp{i}", [128, 4, 128], BF16)[:] for i in range(2)]

        sems = {}
        for e in Prog.ENGS:
            sems[e] = es.enter_context(nc.semaphore("sem_" + e))

        def newslot(name):
            sems[name] = es.enter_context(nc.semaphore("sl_" + name))
            return P.slot(name)

        mm_ring = Ring(ps_mm)
        tp_ring = Ring(ps_tp)

        all_dma = {}

        def dma(q, out, in_, slot, deps=()):
            tok = P.op(q, lambda e, out=out, in_=in_: e.dma_start(out=out, in_=in_), deps, slot=slot)
            all_dma[slot] = tok
            return tok

        def mm(out, lhsT, rhs, start, stop, deps=(), sig=False):
            return P.op("pe", lambda e, out=out, lhsT=lhsT, rhs=rhs, start=start, stop=stop:
                        e.matmul(out, lhsT=lhsT, rhs=rhs, start=start, stop=stop), deps, sig=sig)

        def tpose(out, in_, ident, deps=(), sig=False):
            return P.op("pe", lambda e, out=out, in_=in_, ident=ident: e.transpose(out=out, in_=in_, identity=ident),
                        deps, sig=sig)

        def act(out, in_, func, deps=(), scale=1.0, bias=0.0, accum_out=None):
            def f(e, out=out, in_=in_, func=func, scale=scale, bias=bias, accum_out=accum_out):
                kw = {}
                if accum_out is not None:
                    kw["accum_out"] = accum_out
                np_ = in_.shape[0]
                if bias == 1.0:
                    kw["bias"] = cbias[0:np_, 1:2]
                elif bias == EPS:
                    kw["bias"] = cbias[0:np_, 2:3]
                return e.activation(out=out, in_=in_, func=func, scale=scale, **kw)
            return P.op("act", f, deps)

        def vcopy(eng, out, in_, deps=()):
            if eng == "act":
                return act(out, in_, AF.Copy, deps)
            return P.op(eng, lambda e, out=out, in_=in_: e.tensor_copy(out=out, in_=in_), deps)

        def tt(eng, out, in0, in1, op, deps=()):
            return P.op(eng, lambda e, out=out, in0=in0, in1=in1, op=op: e.tensor_tensor(out=out, in0=in0, in1=in1, op=op), deps)

        def ts(eng, out, in0, s1, op0, s2=None, op1=None, deps=()):
            def f(e, out=out, in0=in0, s1=s1, op0=op0, s2=s2, op1=op1):
                if op1 is None:
                    return e.tensor_scalar(out=out, in0=in0, scalar1=s1, scalar2=None, op0=op0)
                return e.tensor_scalar(out=out, in0=in0, scalar1=s1, scalar2=s2, op0=op0, op1=op1)
            return P.op(eng, f, deps)

        def stt(eng, out, in0, scalar, in1, op0, op1, deps=()):
            return P.op(eng, lambda e, out=out, in0=in0, scalar=scalar, in1=in1, op0=op0, op1=op1:
                        e.scalar_tensor_tensor(out=out, in0=in0, scalar=scalar, in1=in1, op0=op0, op1=op1), deps)

        class _Stop(Exception):
            pass

        dslot = []

        def dump(src_f32, deps):
            if not dslot:
                dslot.append(newslot("dump"))
            n = src_f32.shape[1]
            ov = out_d.rearrange("(p a) e -> p (a e)", p=128)
            st = None
            for c0 in range(0, n, 4096):
                c1 = min(n, c0 + 4096)
                st = dma("sp", ov[:, c0:c1], src_f32[:, c0:c1], dslot[0], [deps, list(all_dma.values())])
            P.op("sp", lambda e: e.nop(), [st], sig=False)
            raise _Stop()

        def plan():
            cs = newslot("consts")
            ctok = None
            for (dst, src) in [(go_col[:], go_col_d), (ropeC, ropeC_d), (ropeS, ropeS_d), (perm[:], perm_d),
                               (mask_sb, mask_sb_d.rearrange("p (j q) -> p j q", j=4)),
                               (mask_mb, mask_mb_d.rearrange("p (j q) -> p j q", j=4)),
                               (gbias[:], gbias_d), (ownb[:], ownb_d), (ident_bf[:], ident_bf_d),
                               (tri[:], tri_d), (negones[:], negones_d), (ones[:], ones_d),
                               (onehot[:], onehot_d.rearrange("p (n k) -> p n k", n=8))]:
                ctok = dma("sp", dst, src, cs)
            gslot = newslot("gbc")
            gtok = dma("sp", gbc[:], g1_bc, gslot)
            P.op("dve", lambda e: e.memset(stat[:], 0.0), sig=False)
            P.op("dve", lambda e: e.memset(cbias[:, 0:1], 0.0), sig=False)
            P.op("dve", lambda e: e.memset(cbias[:, 1:2], 1.0), sig=False)
            t0 = P.op("dve", lambda e: e.memset(cbias[:, 2:3], EPS))
            if stage == 0:
                dump(cC[:, 12288:24576].bitcast(F32), [ctok, gtok, t0])
            if stage == -1:
                dump(stat[:], [t0])

            xs = [newslot("xs0"), newslot("xs1")]
            xfree = [[], []]
            hnfree = [[], []]
            junk_tok = [None]

            def norm_transpose(src_tile_ap, ntok_tiles, ssq, rs, rstd, dstT, gt, load_fn, extra_deps=()):
                last = []
                for tti in range(ntok_tiles):
                    k = tti % 2
                    xin, ltok = load_fn(tti, k, xfree[k])
                    a1 = act(junk, xin, AF.Square, [ltok, junk_tok[0], t0, extra_deps], accum_out=ssq[:, tti:tti + 1])
                    junk_tok[0] = a1
                    if stage == 10:
                        dump(stat[:], [a1])
                    a2 = act(rs[:, tti:tti + 1], ssq[:, tti:tti + 1], AF.Sqrt, [a1], scale=1.0 / D, bias=EPS)
                    if stage == 11:
                        dump(stat[:], [a2])
                    v1 = P.op("dve", lambda e, o=rstd[:, tti:tti + 1], i=rs[:, tti:tti + 1]: e.reciprocal(out=o, in_=i), [a2])
                    v2 = stt("dve", hn[k], xin, rstd[:, tti:tti + 1], gbc[:], ALU.mult, ALU.mult, [v1, gt, hnfree[k], ltok])
                    if stage == 12:
                        dump(stat[:], [v1])
                    if stage == 13:
                        dump(cD[:, 8192:12288].bitcast(F32), [v2])
                    xfree[k] = [a1, v2]
                    tps = []
                    for g in range(4):
                        rk, pt, fr = tp_ring.get()
                        tl = None
                        for j in range(4):
                            c = g * 4 + j
                            tl = tpose(pt[:, j, :], hn[k][:, c * 128:(c + 1) * 128], ident_bf[:],
                                       [v2, fr, ctok] if j == 0 else (), sig=(j == 3))
                        eng = "act"
                        cp = vcopy(eng, dstT[:, g * 4:(g + 1) * 4, tti * 128:(tti + 1) * 128], pt, [tl, extra_deps])
                        if stage == 16 + g:
                            dump(stat[:], [cp])
                        tp_ring.release(rk, [cp])
                        tps.append(tl)
                        last.append(cp)
                    hnfree[k] = tps
                    if stage == 14:
                        dump(arenaA[:, 0:8192].bitcast(F32), last)
                return last

            def load_ctx(tti, k, free):
                tok = dma("sp", xt[k], xc[tti * 128:(tti + 1) * 128, :], xs[k], free)
                return xt[k], tok

            hT_ready = norm_transpose(None, 16, ssq1, rs1, rstd1, hT, gtok, load_ctx)
            p1_done = list(hT_ready)
            if stage == 1:
                dump(arenaA[:].bitcast(F32), p1_done)

            whfree = [[], []]
            wh_tok = {}
            selz = P.op("dve", lambda e: e.memset(selbT, 0.0), [p1_done])

            def load_wh(h):
                k = head_list.index(h) % 2
                deps_ = whfree[k]
                sl_ = newslot(f"wh_{h}")
                wh_tok[h] = dma("pool", wh[k].rearrange("p c n -> p (c n)"), w_in_h[h], sl_, deps_)

            head_list = list(range(NH))
            if stage == 5:
                head_list = [8]
            for h_ in head_list[:2]:
                load_wh(h_)
            qkv_free = list(p1_done)
            acc_free = [[], []]
            small_free = []
            onT_written = []
            ssq_tok = {0: t0, 1: t0}
            evac_i = [0]
            last_proj_mm = None

            ring = mm_ring
            for hi_, h in enumerate(head_list):
                moba = h < 8
                k = hi_ % 2
                if (not moba) and ring is mm_ring:
                    ring = Ring(ps_mm + [ps_acc[1]])
                    ring.free = [list(mm_ring.free[0]), list(mm_ring.free[1]), list(mm_ring.free[2]), [acc_free[1], onT_written[-6:]]]
                W = wh[k]
                wtok = wh_tok[h]
                head_reads = []
                proj_evac = []
                for which, ntile, dst, col0, sc in (("q", 2, QT, 0, SCALE), ("k", 4, KT, 128, 1.0)):
                    for ti in range(ntile):
                        pos0 = (HALF + ti * 512) if which == "q" else ti * 512
                        rk, pm, fr = ring.get()
                        for c in range(16):
                            lt = mm(pm[:], W[:, c, col0:col0 + 128], hT[:, c, pos0:pos0 + 512], c == 0, c == 15,
                                    [wtok, fr, hT_ready] if c == 0 else (), sig=(c == 15))
                        dsl = dst[:, ti * 512:(ti + 1) * 512]
                        if moba:
                            e1 = act(raw32, pm[0:32, :], AF.Copy, [lt, qkv_free], scale=sc)
                            if stage == 20:
                                dump(stat[:], [e1])
                            h1 = vcopy("dve", rhi, raw32, [e1, qkv_free])
                            h2 = tt("dve", rlo, raw32, rhi, ALU.subtract, [h1])
                            rk2, pm2, fr2 = ring.get()
                            mm(pm2[:], perm[:], rhi, True, False, [h1, fr2, ctok])
                            sw = mm(pm2[:], perm[:], rlo, False, True, [h2], sig=True)
                            d1 = tt("dve", ropeA, raw32, ropeC[:, pos0:pos0 + 512], ALU.mult, [e1, ctok])
                            d2 = tt("dve", ropeB, pm2[0:32, :], ropeS[:, pos0:pos0 + 512], ALU.mult, [sw, ctok])
                            if stage == 21:
                                dump(stat[:], [d1, d2])
                            e2 = act(dsl, pm[:], AF.Copy, [lt, qkv_free], scale=sc)
                            d3 = tt("dve", dsl[0:32, :], ropeA, ropeB, ALU.add, [d1, d2, e2, qkv_free])
                            ring.release(rk2, [d2])
                            ring.release(rk, [e1, e2])
                            proj_evac += [d3, e2]
                            qkv_free = [qkv_free, d3]
                        else:
                            evac_i[0] += 1
                            eng = "act" if evac_i[0] % 2 == 0 else "dve"
                            if eng == "act":
                                e2 = act(dsl, pm[:], AF.Copy, [lt, qkv_free], scale=sc)
                            else:
                                e2 = ts("dve", dsl, pm[:], sc, ALU.mult, deps=[lt, qkv_free])
                            ring.release(rk, [e2])
                            proj_evac.append(e2)
                if stage == 23:
                    dump(cD[:, 0:5120].bitcast(F32), [proj_evac])
                for g in range(4):
                    rk, pm, fr = ring.get()
                    for j in range(4):
                        tti = g * 4 + j
                        for c in range(16):
                            lt = mm(pm[:, j * 128:(j + 1) * 128], hT[:, c, tti * 128:(tti + 1) * 128], W[:, c, 256:384],
                                    c == 0, c == 15, [wtok, fr, hT_ready] if (j == 0 and c == 0) else (),
                                    sig=(j == 3 and c == 15))
                    evac_i[0] += 1
                    eng = "act" if evac_i[0] % 2 == 0 else "dve"
                    if stage == 24:
                        eng = "act"
                    if stage == 25:
                        eng = "dve"
                    e2 = vcopy(eng, Vt[:, g * 4:(g + 1) * 4, :], pm[:].rearrange("p (t d) -> p t d", t=4), [lt, qkv_free])
                    if stage in (24, 25) and g == 1:
                        dump(cD[:, 0:5120].bitcast(F32), [proj_evac, e2])
                    if stage == 26 and g == 2:
                        dump(cD[:, 0:5120].bitcast(F32), [proj_evac, e2])
                    if stage == 27 and g == 3:
                        dump(cD[:, 0:5120].bitcast(F32), [proj_evac, e2])
                    ring.release(rk, [e2])
                    proj_evac.append(e2)
                last_proj_mm = lt
                whfree[k] = [lt]
                if stage in (2, 28, 29) and h == 0:
                    dump(cD[:, 0:5120].bitcast(F32), [proj_evac])
                if hi_ + 2 < len(head_list):
                    load_wh(head_list[hi_ + 2])

                if moba:
                    r1 = P.op("dve", lambda e: e.tensor_reduce(out=km, in_=KT.rearrange("p (n k) -> p n k", n=8),
                                                               axis=AX.X, op=ALU.add), [proj_evac])
                    r2 = ts("dve", kms, km, 1.0 / 256.0, ALU.mult, deps=[r1])
                    r3 = vcopy("dve", km_hi, kms, [r2])
                    r4 = tt("dve", km_lo, kms, km_hi, ALU.subtract, [r3])
                    for j in range(8):
                        mm(ps_small[:, j * 8:(j + 1) * 8], QT[:, j * 128:(j + 1) * 128], km_hi, True, False,
                           [r4, proj_evac, small_free] if j == 0 else ())
                        gl = mm(ps_small[:, j * 8:(j + 1) * 8], QT[:, j * 128:(j + 1) * 128], km_lo, False, True, sig=(j == 7))
                    g1 = tt("dve", gm, ps_small[:, 0:64], gbias[:], ALU.add, [gl, ctok])
                    small_free = [g1]
                    gx = g1
                    for j in range(8):
                        gx = P.op("dve", lambda e, o=top[:, j * 8:(j + 1) * 8], i=gm[:, j * 8:(j + 1) * 8]: e.max(out=o, in_=i), [gx])
                    for j in range(8):
                        gx = ts("dve", sel[:, j * 8:(j + 1) * 8], gm[:, j * 8:(j + 1) * 8], top[:, j * 8 + 3:j * 8 + 4],
                                ALU.is_gt, deps=[gx])
                    gx = tt("dve", sel, sel, ownb[:], ALU.add, [gx])
                    gx = ts("dve", selb[:], sel, -1.0, ALU.add, NEGBIG, ALU.mult, deps=[gx])
                    sel_cp = []
                    for half_ in range(2):
                        rk, pt, fr = tp_ring.get()
                        for j in range(4):
                            jj = half_ * 4 + j
                            tl = tpose(pt[0:8, j, :], selb[:, jj * 8:(jj + 1) * 8], ident_bf[:],
                                       [gx, fr, ctok] if j == 0 else (), sig=(j == 3))
                        cp = act(selbT[0:8, half_ * 512:(half_ + 1) * 512], pt[0:8].rearrange("p j t -> p (j t)"), AF.Copy,
                                 [tl, qkv_free, selz])
                        tp_ring.release(rk, [cp])
                        sel_cp.append(cp)

                if stage == 3 and h == 0:
                    dump(gsm[:].rearrange("p a b -> p (a b)"), [gx, sel_cp])
                for Qi in range(2):
                    qs = QT[:, Qi * 512:(Qi + 1) * 512]
                    qsl = slice(Qi * 512, (Qi + 1) * 512)
                    last_kt = 8 + 4 * Qi + 3
                    accO = ps_acc[0]
                    accD = ps_acc[1]
                    d0 = 8 + 4 * Qi
                    if moba:
                        kts = list(range(0, last_kt + 1))
                        n = len(kts)
                        st_, pv_tok = {}, {}

                        def A_(idx):
                            kt = kts[idx]
                            r = idx % 2
                            rk, pm, fr = ring.get()
                            mm(pm[:], KT[:, kt * 128:(kt + 1) * 128], qs, True, False, [proj_evac, fr, sel_cp])
                            s1 = mm(pm[:], onehot[:, kt // 2, :], selbT[:, qsl], False, True, sig=True)
                            e1 = act(Ab[r], pm[:], AF.Exp, [s1, pv_tok.get(idx - 2)])
                            ring.release(rk, [e1])
                            pt = e1
                            if kt >= d0:
                                pt = tt("dve", Ab[r], Ab[r], mask_mb[:, kt - d0, :], ALU.mult, [e1, ctok])
                            st_[idx] = pt

                        def B_(idx):
                            kt = kts[idx]
                            r = idx % 2
                            first = idx == 0
                            lastk = idx == n - 1
                            mm(accO[:], Vt[:, kt, :], Ab[r], first, lastk, [st_[idx], acc_free[0]] if first else [st_[idx]])
                            pv_tok[idx] = mm(accD[:], ones[:], Ab[r], first, lastk, [acc_free[1]] if first else (), sig=True)

                        A_(0)
                        for idx in range(n):
                            if idx + 1 < n:
                                A_(idx + 1)
                            B_(idx)
                        fin = pv_tok[n - 1]
                    else:
                        kts = list(range(last_kt, -1, -1))
                        n = len(kts)
                        ztok, zbuf, ltok_, gtok_, gbuf, rtoks, rbuf, xtok, vtok = {}, {}, {}, {}, {}, {}, {}, {}, {}

                        def Z_(idx):
                            kt = kts[idx]
                            rk, pm, fr = ring.get()
                            ztok[idx] = mm(pm[:], KT[:, kt * 128:(kt + 1) * 128], qs, True, True, [proj_evac, fr], sig=True)
                            zbuf[idx] = (rk, pm)

                        def E_(idx):
                            kt = kts[idx]
                            r = idx % 2
                            rk, pm = zbuf[idx]
                            e1 = act(Ef[r], pm[:], AF.Exp, [ztok[idx]])
                            ring.release(rk, [e1])
                            e2 = act(Lp[r], Ef[r], AF.Ln, [e1, gtok_.get(idx - 2), rtoks.get(idx - 2)], bias=1.0)
                            lt_ = e2
                            if kt >= d0:
                                lt_ = tt("dve", Lp[r], Lp[r], mask_sb[:, kt - d0, :], ALU.mult, [e2, ctok])
                            ltok_[idx] = lt_

                        def G_(idx):
                            kt = kts[idx]
                            r = idx % 2
                            first = idx == 0
                            rk2, pg, fr2 = ring.get()
                            mm(pg[:], KT[:, kt * 128:(kt + 1) * 128], qs, True, False, [fr2, proj_evac])
                            g2 = mm(pg[:], tri[:], Lp[r], False, first, [ltok_[idx], ctok], sig=first)
                            if not first:
                                g2 = mm(pg[:], negones[:], Rb[rbuf[idx - 1]], False, True, [rtoks[idx - 1]], sig=True)
                            gtok_[idx] = g2
                            gbuf[idx] = (rk2, pg)

                        def R_(idx):
                            if idx == n - 1:
                                return
                            r = idx % 2
                            if idx == 0:
                                rtoks[0] = vcopy("dve", Rb[0], Lp[r], [ltok_[0]])
                                rbuf[0] = 0
                            else:
                                nb = 1 - rbuf[idx - 1]
                                rtoks[idx] = tt("dve", Rb[nb], Rb[rbuf[idx - 1]], Lp[r], ALU.add,
                                                [ltok_[idx], rtoks[idx - 1], gtok_.get(idx - 1)])
                                rbuf[idx] = nb

                        def X_(idx):
                            kt = kts[idx]
                            r = idx % 2
                            rk2, pg = gbuf[idx]
                            e3 = act(Ab[r], pg[:], AF.Exp, [gtok_[idx], vtok.get(idx - 2)])
                            ring.release(rk2, [e3])
                            at_ = e3
                            if kt >= d0:
                                at_ = tt("dve", Ab[r], Ab[r], mask_sb[:, kt - d0, :], ALU.mult, [e3])
                            xtok[idx] = at_

                        def V_(idx):
                            kt = kts[idx]
                            r = idx % 2
                            first = idx == 0
                            lastk = idx == n - 1
                            vtok[idx] = mm(accO[:], Vt[:, kt, :], Ab[r], first, lastk,
                                           [xtok[idx], acc_free[0]] if first else [xtok[idx]], sig=True)

                        Z_(0)
                        Z_(1)
                        E_(0)
                        for idx in range(n):
                            if idx + 2 < n:
                                Z_(idx + 2)
                            if idx + 1 < n:
                                E_(idx + 1)
                            G_(idx)
                            R_(idx)
                            X_(idx)
                            V_(idx)
                        fin = vtok[n - 1]
                    head_reads = [fin]
                    if moba:
                        v1 = P.op("dve", lambda e: e.reciprocal(out=rden, in_=accD[:]), [fin, onT_written[-2:]])
                        v2 = tt("dve", Of, accO[:], rden, ALU.mult, [v1])
                        acc_free = [[v2], [v1]]
                    else:
                        v2 = vcopy("dve", Of, accO[:], [fin, onT_written[-2:]])
                        acc_free = [[v2], acc_free[1]]
                    a1 = act(onT[:, h, Qi * 512:(Qi + 1) * 512], Of, AF.Copy, [v2, ctok], scale=go_col[:, h:h + 1])
                    a2 = act(sqb, Of, AF.Square, [v2, onT_written[-2:]])
                    for j in range(4):
                        sl = mm(ps_small[:, 64 + j:65 + j], sqb[:, j * 128:(j + 1) * 128], ones[:, 0:1], True, True,
                                [a2, small_free] if j == 0 else (), sig=(j == 3))
                    gi = 0 if moba else 1
                    sacc = ssqA if moba else ssqB
                    v3 = tt("dve", sacc[:, Qi * 4:(Qi + 1) * 4], sacc[:, Qi * 4:(Qi + 1) * 4], ps_small[:, 64:68], ALU.add,
                            [sl, ssq_tok[gi]])
                    ssq_tok[gi] = v3
                    small_free = [small_free, v3]
                    onT_written += [a1, a2, sl]
                    head_reads = [head_reads, a1, a2]
                qkv_free = [head_reads, onT_written[-6:]]
                if (stage == 4 and h == 0) or (stage == 5 and h == 8):
                    dump(arenaB[:, h * 1024:(h + 1) * 1024].bitcast(F32), [onT_written, qkv_free, ssq_tok[0], ssq_tok[1]])

            att_done = [onT_written, ssq_tok[0], ssq_tok[1], qkv_free]
            if ring is not mm_ring:
                for kk_ in range(3):
                    mm_ring.free[kk_] = [ring.free[kk_], ring.free[3]]

            x1s = newslot("x1ld")
            x1tok = None
            for tti in range(8):
                x1tok = dma("sp", x1[:, tti, :], xc[HALF + tti * 128:HALF + (tti + 1) * 128, :], x1s, [last_proj_mm, att_done])
            g2tok = dma("sp", gbc[:], g2_bc, gslot, [p1_done, att_done])
            a_ = act(rsAB, stat[:, 48:64], AF.Sqrt, [att_done], scale=1.0 / 1024.0, bias=EPS)
            rAB = P.op("dve", lambda e: e.reciprocal(out=rstdAB, in_=rsAB), [a_])
            wofree = [[att_done], [att_done]]
            wo_tok = {}

            def load_wo(et):
                kk = et % 2
                sl_ = newslot(f"wo_{et}")
                for hh in range(2):
                    wo_tok[et] = dma("pool", wo[kk][:, hh * 8:(hh + 1) * 8, :].rearrange("p h e -> p (h e)"),
                                     w_out_r[et][:, hh * 4096:(hh + 1) * 4096], sl_, wofree[kk])

            load_wo(0)
            load_wo(1)
            x1_last = {}
            for et in range(4):
                kk = et % 2
                for tti in range(8):
                    toks = []
                    for gi in range(2):
                        rk, pm, fr = mm_ring.get()
                        for hh in range(8):
                            h = gi * 8 + hh
                            lt = mm(pm[:], onT[:, h, tti * 128:(tti + 1) * 128], wo[kk][:, h, :], hh == 0, hh == 7,
                                    [wo_tok[et], fr, att_done] if hh == 0 else (), sig=(hh == 7))
                        xsl = x1[:, tti, et * 512:(et + 1) * 512]
                        v = stt("dve", xsl, pm[:], rstdAB[:, gi * 8 + tti:gi * 8 + tti + 1], xsl, ALU.mult, ALU.add,
                                [lt, rAB, x1tok, x1_last.get((tti, et))])
                        x1_last[(tti, et)] = v
                        mm_ring.release(rk, [v])
                        toks.append(lt)
                wofree[kk] = [lt]
                if et + 2 < 4:
                    load_wo(et + 2)
            op_done = [x1_last[kk_] for kk_ in x1_last] + [lt]
            if stage == 6:
                dump(arenaA[:].bitcast(F32), op_done)

            def load_x1(tti, k, free):
                return x1[:, tti, :], None

            h2_ready = norm_transpose(None, 8, ssq2, rs2, rstd2, h2T, g2tok, load_x1, extra_deps=op_done)

            if stage == 7:
                dump(arenaB[:].bitcast(F32), h2_ready)
            wufree = [[op_done], [op_done]]
            wdfree = [[op_done]]
            wu_tok = {}
            wd_tok = {}

            def load_wu(fb):
                kk = fb % 2
                sl_ = newslot(f"wu_{fb}")
                for fc in range(4):
                    wu_tok[fb] = dma("pool", wu[kk][:, fc].rearrange("p c n -> p (c n)"),
                                     w_up_r[fb][:, fc * 2048:(fc + 1) * 2048], sl_, wufree[kk])

            def load_wd(fb):
                wd_tok[fb] = dma("pool", wd1, w_down[fb * 512:(fb + 1) * 512, :].rearrange("(c p) e -> p c e", p=128),
                                 newslot(f"wd_{fb}"), wdfree[0])

            load_wu(0)
            load_wd(0)
            load_wu(1)
            ATfree = [[], []]
            rlfree = [[], []]
            rli = 0
            for fb in range(16):
                kk = fb % 2
                at_w = []
                for fc in range(4):
                    for tq in range(2):
                        rk, pm, fr = mm_ring.get()
                        for c in range(16):
                            lt = mm(pm[:], wu[kk][:, fc, c, :], h2T[:, c, tq * 512:(tq + 1) * 512], c == 0, c == 15,
                                    [wu_tok[fb], fr, h2_ready] if c == 0 else (), sig=(c == 15))
                        ri = rli % 2
                        rli += 1
                        e1 = act(rls[ri][:], pm[:], AF.Relu, [lt, rlfree[ri]])
                        mm_ring.release(rk, [e1])
                        v = tt("dve", ATs[kk][:, fc, tq * 512:(tq + 1) * 512], rls[ri][:], rls[ri][:], ALU.mult, [e1, ATfree[kk]])
                        rlfree[ri] = [v]
                        at_w.append(v)
                wufree[kk] = [lt]
                if fb + 2 < 16:
                    load_wu(fb + 2)
                for tti in range(8):
                    for et in range(4):
                        rk, pm, fr = mm_ring.get()
                        for fc in range(4):
                            lt = mm(pm[:], ATs[kk][:, fc, tti * 128:(tti + 1) * 128], wd1[:, fc, et * 512:(et + 1) * 512],
                                    fc == 0, fc == 3, [wd_tok[fb], fr, at_w] if fc == 0 else (), sig=(fc == 3))
                        xsl = x1[:, tti, et * 512:(et + 1) * 512]
                        v = tt("dve", xsl, xsl, pm[:], ALU.add, [lt, x1_last.get((tti, et)), h2_ready])
                        x1_last[(tti, et)] = v
                        mm_ring.release(rk, [v])
                wdfree[0] = [lt]
                ATfree[kk] = [lt]
                if fb + 1 < 16:
                    load_wd(fb + 1)

            mlp_done = [x1_last[kk_] for kk_ in x1_last]
            gftok = dma("sp", gbc[:], gf_bc, gslot, [h2_ready])
            osl = [newslot("os0"), newslot("os1")]
            ofree = [[mlp_done], [mlp_done]]
            store_toks = []
            for tti in range(8):
                k = tti % 2
                a1 = act(junk, x1[:, tti, :], AF.Square, [mlp_done, junk_tok[0]], accum_out=ssq3[:, tti:tti + 1])
                junk_tok[0] = a1
                a2 = act(rs3[:, tti:tti + 1], ssq3[:, tti:tti + 1], AF.Sqrt, [a1], scale=1.0 / D, bias=EPS)
                v1 = P.op("dve", lambda e, o=rstd3[:, tti:tti + 1], i=rs3[:, tti:tti + 1]: e.reciprocal(out=o, in_=i), [a2])
                v2 = stt("dve", yo[k], x1[:, tti, :], rstd3[:, tti:tti + 1], gbc[:], ALU.mult, ALU.mult, [v1, gftok, ofree[k]])
                st = dma("sp", out_d[tti * 128:(tti + 1) * 128, :], yo[k], osl[k], [v2])
                ofree[k] = [st]
                store_toks.append(st)
            P.op("sp", lambda e: e.nop(), store_toks, sig=False)

        try:
            plan()
        except _Stop:
            pass

        engmap = {"pe": "tensor", "act": "scalar", "dve": "vector", "pool": "gpsimd", "sp": "sync"}
        with nc.Block() as block:
            for ename in Prog.ENGS:
                def body(e, ename=ename):
                    for (waits, fn, inc) in P.q[ename]:
                        for (key, val) in waits:
                            e.wait_ge(sems[key], val)
                        ins = fn(e)
                        if inc is not None:
                            ins.then_inc(sems[inc[0]], inc[1])
                getattr(block, engmap[ename])(body)
    return nc


_NC_CACHE = {}


def _bf(a):
    return np.ascontiguousarray(a.astype(ml_dtypes.bfloat16))


def make_in_maps(x, mix_norm_g, w_in, moba_out_g, sb_out_g, w_out, mlp_norm_g, w_up, w_down, final_norm_g):
    x = np.asarray(x, dtype=np.float32)
    w_in = np.asarray(w_in, dtype=np.float32)[0]
    w_out = np.asarray(w_out, dtype=np.float32)[0]
    w_up = np.asarray(w_up, dtype=np.float32)[0]
    w_down_ = np.ascontiguousarray(np.asarray(w_down, dtype=np.float32)[0])
    B = x.shape[0]

    heads = []
    for h in range(NH):
        if h < 8:
            qc, kc, vc = h * 128, 1024 + h * 128, 2048 + h * 128
        else:
            hh = h - 8
            qc, kc, vc = 3072 + hh * 128, 4096 + hh * 128, 5120 + hh * 128
        sl = np.concatenate([w_in[:, qc:qc + 128], w_in[:, kc:kc + 128], w_in[:, vc:vc + 128]], axis=1)
        heads.append(sl.reshape(16, 128, 384).transpose(1, 0, 2).reshape(128, 16 * 384))
    w_in_h = np.ascontiguousarray(np.stack(heads, 0))
    w_out_r = np.ascontiguousarray(
        w_out.reshape(16, 128, 4, 512).transpose(2, 1, 0, 3).reshape(4, 128, 16 * 512))
    w_up_r = np.ascontiguousarray(
        w_up.reshape(16, 128, 16, 4, 128).transpose(2, 1, 3, 0, 4).reshape(16, 128, 4 * 16 * 128))

    def bc(v):
        return np.ascontiguousarray(np.broadcast_to(np.asarray(v, np.float32).reshape(1, D), (128, D)))

    g1 = bc(np.asarray(mix_norm_g)[0])
    g2 = bc(np.asarray(mlp_norm_g)[0])
    gf = bc(np.asarray(final_norm_g))
    go = np.concatenate([np.asarray(moba_out_g, np.float32)[0], np.asarray(sb_out_g, np.float32)[0]])
    go_col = np.ascontiguousarray(go.reshape(16, 128).T)

    kk = np.arange(128)[:, None]
    qq = np.arange(512)[None, :]
    msb = np.stack([((128 * j + kk) < qq) for j in range(4)], 1).astype(np.float32)
    mmb = np.stack([((128 * j + kk) <= qq) for j in range(4)], 1).astype(np.float32)
    mask_sb = _bf(msb.reshape(128, 2048))
    mask_mb = _bf(mmb.reshape(128, 2048))
    perm = np.zeros((32, 128), np.float32)
    for m in range(32):
        perm[(m + 16) % 32, m] = 1.0
    ident = np.eye(128, dtype=np.float32)
    tri = np.where(np.arange(128)[:, None] >= np.arange(128)[None, :], -1.0, 0.0).astype(np.float32)
    onehot = np.zeros((128, 8, 128), np.float32)
    for n in range(8):
        onehot[n, n, :] = 1.0
    inv_freq = (np.float32(500000.0) ** (-np.arange(16, dtype=np.float32) / np.float32(16))).astype(np.float32)

    in_maps = []
    for c in range(2 * B):
        b, half = c // 2, c % 2
        if half == 1:
            xcx = np.ascontiguousarray(x[b])
            pos = np.arange(S, dtype=np.float32)
        else:
            xcx = np.concatenate([np.zeros((HALF, D), np.float32), x[b, :HALF]], 0)
            pos = np.maximum(np.arange(S, dtype=np.float32) - HALF, 0).astype(np.float32)
        ang = pos[None, :] * inv_freq[:, None]
        cos = np.cos(ang).astype(np.float32)
        sin = np.sin(ang).astype(np.float32)
        ropeC = np.concatenate([cos, cos], 0)
        ropeS = np.concatenate([-sin, sin], 0)
        gb = np.zeros((128, 8, 8), np.float32)
        ob = np.zeros((128, 8, 8), np.float32)
        for j in range(8):
            qblk = 4 + j // 2
            for n in range(8):
                elig = (n < qblk) and (half == 1 or n >= 4)
                gb[:, j, n] = 0.0 if elig else -1e30
            ob[:, j, qblk] = 1.0
        in_maps.append({
            "xc": np.ascontiguousarray(xcx), "w_in_h": w_in_h, "w_out_r": w_out_r, "w_up_r": w_up_r, "w_down": w_down_,
            "g1_bc": g1, "g2_bc": g2, "gf_bc": gf, "go_col": go_col,
            "ropeC": np.ascontiguousarray(ropeC), "ropeS": np.ascontiguousarray(ropeS), "perm": _bf(perm),
            "mask_sb": mask_sb, "mask_mb": mask_mb,
            "gbias": np.ascontiguousarray(gb.reshape(128, 64)), "ownb": np.ascontiguousarray(ob.reshape(128, 64)),
            "ident_bf": _bf(ident), "tri": _bf(tri), "negones": _bf(-np.ones((128, 128), np.float32)),
            "ones": _bf(np.ones((128, 128), np.float32)), "onehot": _bf(onehot.reshape(128, 1024)),
        })

    return in_maps


def kernel(x, mix_norm_g, w_in, moba_out_g, sb_out_g, w_out, mlp_norm_g, w_up, w_down, final_norm_g):
    x = np.asarray(x, dtype=np.float32)
    B = x.shape[0]
    in_maps = make_in_maps(x, mix_norm_g, w_in, moba_out_g, sb_out_g, w_out, mlp_norm_g, w_up, w_down, final_norm_g)
    if "nc" not in _NC_CACHE:
        _NC_CACHE["nc"] = build_program()
    nc = _NC_CACHE["nc"]
    res = run_bass_kernel_spmd(nc, in_maps, core_ids=list(range(2 * B)))
    out = np.zeros((B, S, D), np.float32)
    for c in range(2 * B):
        b, half = c // 2, c % 2
        out[b, half * HALF:(half + 1) * HALF] = np.asarray(res.results[c]["out"], dtype=np.float32)
    return out
```

```python
from contextlib import ExitStack

import numpy as np
import ml_dtypes
import concourse.bass as bass
import concourse.mybir as mybir
from concourse.bass_utils import run_bass_kernel_spmd

F32 = mybir.dt.float32
BF16 = mybir.dt.bfloat16
AF = mybir.ActivationFunctionType
ALU = mybir.AluOpType
AX = mybir.AxisListType

D = 2048
S = 2048
HALF = 1024
NH = 16
EPS = 1e-6
SCALE = 128 ** -0.5
NEGBIG = 30000.0


class Prog:
    ENGS = ("pe", "act", "dve", "pool", "sp")

    def __init__(self):
        self.q = {e: [] for e in self.ENGS}
        self.cnt = {e: 0 for e in self.ENGS}
        self.known = {e: {} for e in self.ENGS}
        self.slots = []

    def slot(self, name):
        self.cnt[name] = 0
        self.slots.append(name)
        return name

    def op(self, eng, fn, deps=(), sig=True, slot=None):
        waits = []
        for d in self._flat(deps):
            key, val = d
            if key == "pe" and eng == "pe":
                continue
            if self.known[eng].get(key, 0) < val:
                waits.append((key, val))
                self.known[eng][key] = val
        if slot is not None:
            self.cnt[slot] += 16
            tok = (slot, self.cnt[slot])
            inc = (slot, 16)
        elif sig:
            self.cnt[eng] += 1
            tok = (eng, self.cnt[eng])
            inc = (eng, 1)
        else:
            tok = None
            inc = None
        self.q[eng].append((waits, fn, inc))
        return tok

    def _flat(self, deps):
        out = []
        if deps is None:
            return out
        if isinstance(deps, tuple) and len(deps) == 2 and isinstance(deps[0], str):
            return [deps]
        for d in deps:
            out.extend(self._flat(d))
        return out


class Ring:
    def __init__(self, bufs):
        self.bufs = bufs
        self.free = [[] for _ in bufs]
        self.i = 0

    def get(self):
        k = self.i % len(self.bufs)
        self.i += 1
        return k, self.bufs[k], self.free[k]

    def release(self, k, toks):
        self.free[k] = toks


def build_program(stage=99):
    nc = bass.Bass("TRN2", target_bir_lowering=False)
    P = Prog()

    def din(name, shape, dt=F32):
        return nc.dram_tensor(name, list(shape), dt, kind="ExternalInput").ap()

    xc = din("xc", [S, D])
    w_in_h = din("w_in_h", [NH, 128, 16 * 384])
    w_out_r = din("w_out_r", [4, 128, 16 * 512])
    w_up_r = din("w_up_r", [16, 128, 4 * 16 * 128])
    w_down = din("w_down", [8192, D])
    g1_bc = din("g1_bc", [128, D])
    g2_bc = din("g2_bc", [128, D])
    gf_bc = din("gf_bc", [128, D])
    go_col_d = din("go_col", [128, NH])
    ropeC_d = din("ropeC", [32, S])
    ropeS_d = din("ropeS", [32, S])
    perm_d = din("perm", [32, 128], BF16)
    mask_sb_d = din("mask_sb", [128, 4 * 512], BF16)
    mask_mb_d = din("mask_mb", [128, 4 * 512], BF16)
    gbias_d = din("gbias", [128, 64])
    ownb_d = din("ownb", [128, 64])
    ident_bf_d = din("ident_bf", [128, 128], BF16)
    tri_d = din("tri", [128, 128], BF16)
    negones_d = din("negones", [128, 128], BF16)
    ones_d = din("ones", [128, 128], BF16)
    onehot_d = din("onehot", [128, 8 * 128], BF16)
    out_d = nc.dram_tensor("out", [HALF, D], F32, kind="ExternalOutput").ap()

    es = ExitStack()
    with es:
        def sb(name, shape, dt):
            return es.enter_context(nc.sbuf_tensor(name, list(shape), dt))

        def ps(name, shape, dt):
            return es.enter_context(nc.psum_tensor(name, list(shape), dt))

        arenaA = sb("arenaA", [128, 32768], BF16)
        hT = arenaA[:].rearrange("p (c t) -> p c t", c=16)
        x1 = arenaA[:].bitcast(F32).rearrange("p (t e) -> p t e", t=8)
        arenaB = sb("arenaB", [128, 16384], BF16)
        onT = arenaB[:].rearrange("p (h t) -> p h t", h=16)
        h2T = onT
        arenaC = sb("arenaC", [128, 24576], BF16)
        cC = arenaC[:]
        wh = [cC[:, i * 6144:(i + 1) * 6144].rearrange("p (c n) -> p c n", c=16) for i in range(2)]
        ropeC = cC[0:32, 12288:16384].bitcast(F32)
        ropeS = cC[0:32, 16384:20480].bitcast(F32)
        mask_sb = cC[:, 20480:22528].rearrange("p (j q) -> p j q", j=4)
        mask_mb = cC[:, 22528:24576].rearrange("p (j q) -> p j q", j=4)
        wo = [cC[:, i * 8192:(i + 1) * 8192].rearrange("p (h e) -> p h e", h=16) for i in range(2)]
        wu = [cC[:, i * 8192:(i + 1) * 8192].rearrange("p (f c n) -> p f c n", f=4, c=16) for i in range(2)]
        wd1 = cC[:, 16384:24576].rearrange("p (f e) -> p f e", f=4)
        arenaD = sb("arenaD", [128, 20480], BF16)
        cD = arenaD[:]
        xt = [cD[:, i * 4096:(i + 1) * 4096].bitcast(F32) for i in range(2)]
        hn = [cD[:, 8192 + i * 2048:8192 + (i + 1) * 2048] for i in range(2)]
        junk = cD[:, 12288:14336]
        gbc = cD[:, 14336:18432].bitcast(F32)
        yo = xt
        QT = cD[:, 0:1024]
        KT = cD[:, 1024:3072]
        Vt = cD[:, 3072:5120].rearrange("p (t d) -> p t d", t=16)
        Ef = [cD[:, 5120 + i * 1024:5120 + (i + 1) * 1024].bitcast(F32) for i in range(2)]
        Lp = [cD[:, 7168 + i * 512:7168 + (i + 1) * 512] for i in range(2)]
        Ab = [cD[:, 8192 + i * 512:8192 + (i + 1) * 512] for i in range(2)]
        Rb = [cD[:, 9216 + i * 512:9216 + (i + 1) * 512] for i in range(2)]
        Of = cD[:, 10240:11264].bitcast(F32)
        rden = cD[:, 11264:12288].bitcast(F32)
        sqb = cD[:, 12288:12800]
        raw32 = cD[0:32, 12800:13824].bitcast(F32)
        ropeA = cD[0:32, 13824:14848].bitcast(F32)
        ropeB = cD[0:32, 14848:15872].bitcast(F32)
        selbT = cD[:, 17920:18944]
        rhi = cD[0:32, 16896:17408]
        rlo = cD[0:32, 17408:17920]
        ATs = [cD[:, i * 4096:(i + 1) * 4096].rearrange("p (f t) -> p f t", f=4) for i in range(2)]
        rls = [cD[:, 8192 + i * 1024:8192 + (i + 1) * 1024].bitcast(F32) for i in range(2)]

        go_col = sb("go_col_s", [128, NH], F32)
        perm = sb("perm_s", [32, 128], BF16)
        gbias = sb("gbias_s", [128, 64], F32)
        ownb = sb("ownb_s", [128, 64], F32)
        ident_bf = sb("ident_bf_s", [128, 128], BF16)
        tri = sb("tri_s", [128, 128], BF16)
        negones = sb("negones_s", [128, 128], BF16)
        ones = sb("ones_s", [128, 128], BF16)
        onehot = sb("onehot_s", [128, 8, 128], BF16)
        cbias = sb("cbias", [128, 4], F32)
        stat = sb("stat", [128, 256], F32)
        ssq1 = stat[:, 0:16]
        rs1 = stat[:, 16:32]
        rstd1 = stat[:, 32:48]
        ssqA = stat[:, 48:56]
        ssqB = stat[:, 56:64]
        rsAB = stat[:, 64:80]
        rstdAB = stat[:, 80:96]
        ssq2 = stat[:, 96:104]
        rs2 = stat[:, 104:112]
        rstd2 = stat[:, 112:120]
        ssq3 = stat[:, 120:128]
        rs3 = stat[:, 128:136]
        rstd3 = stat[:, 136:144]
        km = stat[:, 144:152]
        kms = stat[:, 152:160]
        gsm = sb("gsm", [128, 3, 64], F32)
        gm = gsm[:, 0, :]
        top = gsm[:, 1, :]
        sel = gsm[:, 2, :]
        selb = sb("selb", [128, 64], BF16)
        kmb = sb("kmb", [128, 16], BF16)
        km_hi = kmb[:, 0:8]
        km_lo = kmb[:, 8:16]

        ps_mm = [ps(f"ps_mm{i}", [128, 512], F32) for i in range(3)]
        ps_acc = [ps(f"ps_acc{i}", [128, 512], F32) for i in range(2)]
        ps_small = ps("ps_small", [128, 512], F32)
        ps_tp = [ps(f"ps_tp{i}", [128, 4, 128], BF16)[:] for i in range(2)]

        sems = {}
        for e in Prog.ENGS:
            sems[e] = es.enter_context(nc.semaphore("sem_" + e))

        def newslot(name):
            sems[name] = es.enter_context(nc.semaphore("sl_" + name))
            return P.slot(name)

        mm_ring = Ring(ps_mm)
        tp_ring = Ring(ps_tp)

        all_dma = {}

        def dma(q, out, in_, slot, deps=()):
            tok = P.op(q, lambda e, out=out, in_=in_: e.dma_start(out=out, in_=in_), deps, slot=slot)
            all_dma[slot] = tok
            return tok

        def mm(out, lhsT, rhs, start, stop, deps=(), sig=False):
            return P.op("pe", lambda e, out=out, lhsT=lhsT, rhs=rhs, start=start, stop=stop:
                        e.matmul(out, lhsT=lhsT, rhs=rhs, start=start, stop=stop), deps, sig=sig)

        def tpose(out, in_, ident, deps=(), sig=False):
            return P.op("pe", lambda e, out=out, in_=in_, ident=ident: e.transpose(out=out, in_=in_, identity=ident),
                        deps, sig=sig)

        def act(out, in_, func, deps=(), scale=1.0, bias=0.0, accum_out=None):
            def f(e, out=out, in_=in_, func=func, scale=scale, bias=bias, accum_out=accum_out):
                kw = {}
                if accum_out is not None:
                    kw["accum_out"] = accum_out
                np_ = in_.shape[0]
                if bias == 1.0:
                    kw["bias"] = cbias[0:np_, 1:2]
                elif bias == EPS:
                    kw["bias"] = cbias[0:np_, 2:3]
                return e.activation(out=out, in_=in_, func=func, scale=scale, **kw)
            return P.op("act", f, deps)

        def vcopy(eng, out, in_, deps=()):
            if eng == "act":
                return act(out, in_, AF.Copy, deps)
            return P.op(eng, lambda e, out=out, in_=in_: e.tensor_copy(out=out, in_=in_), deps)

        def tt(eng, out, in0, in1, op, deps=()):
            return P.op(eng, lambda e, out=out, in0=in0, in1=in1, op=op: e.tensor_tensor(out=out, in0=in0, in1=in1, op=op), deps)

        def ts(eng, out, in0, s1, op0, s2=None, op1=None, deps=()):
            def f(e, out=out, in0=in0, s1=s1, op0=op0, s2=s2, op1=op1):
                if op1 is None:
                    return e.tensor_scalar(out=out, in0=in0, scalar1=s1, scalar2=None, op0=op0)
                return e.tensor_scalar(out=out, in0=in0, scalar1=s1, scalar2=s2, op0=op0, op1=op1)
            return P.op(eng, f, deps)

        def stt(eng, out, in0, scalar, in1, op0, op1, deps=()):
            return P.op(eng, lambda e, out=out, in0=in0, scalar=scalar, in1=in1, op0=op0, op1=op1:
                        e.scalar_tensor_tensor(out=out, in0=in0, scalar=scalar, in1=in1, op0=op0, op1=op1), deps)

        class _Stop(Exception):
            pass

        dslot = []

        def dump(src_f32, deps):
            if not dslot:
                dslot.append(newslot("dump"))
            n = src_f32.shape[1]
            ov = out_d.rearrange("(p a) e -> p (a e)", p=128)
            st = None
            for c0 in range(0, n, 4096):
                c1 = min(n, c0 + 4096)
                st = dma("sp", ov[:, c0:c1], src_f32[:, c0:c1], dslot[0], [deps, list(all_dma.values())])
            P.op("sp", lambda e: e.nop(), [st], sig=False)
            raise _Stop()

        def plan():
            cs = newslot("consts")
            ctok = None
            for (dst, src) in [(go_col[:], go_col_d), (ropeC, ropeC_d), (ropeS, ropeS_d), (perm[:], perm_d),
                               (mask_sb, mask_sb_d.rearrange("p (j q) -> p j q", j=4)),
                               (mask_mb, mask_mb_d.rearrange("p (j q) -> p j q", j=4)),
                               (gbias[:], gbias_d), (ownb[:], ownb_d), (ident_bf[:], ident_bf_d),
                               (tri[:], tri_d), (negones[:], negones_d), (ones[:], ones_d),
                               (onehot[:], onehot_d.rearrange("p (n k) -> p n k", n=8))]:
                ctok = dma("sp", dst, src, cs)
            gslot = newslot("gbc")
            gtok = dma("sp", gbc[:], g1_bc, gslot)
            P.op("dve", lambda e: e.memset(stat[:], 0.0), sig=False)
            P.op("dve", lambda e: e.memset(cbias[:, 0:1], 0.0), sig=False)
            P.op("dve", lambda e: e.memset(cbias[:, 1:2], 1.0), sig=False)
            t0 = P.op("dve", lambda e: e.memset(cbias[:, 2:3], EPS))
            if stage == 0:
                dump(cC[:, 12288:24576].bitcast(F32), [ctok, gtok, t0])
            if stage == -1:
                dump(stat[:], [t0])

            xs = [newslot("xs0"), newslot("xs1")]
            xfree = [[], []]
            hnfree = [[], []]
            junk_tok = [None]

            def norm_transpose(src_tile_ap, ntok_tiles, ssq, rs, rstd, dstT, gt, load_fn, extra_deps=()):
                last = []
                for tti in range(ntok_tiles):
                    k = tti % 2
                    xin, ltok = load_fn(tti, k, xfree[k])
                    a1 = act(junk, xin, AF.Square, [ltok, junk_tok[0], t0, extra_deps], accum_out=ssq[:, tti:tti + 1])
                    junk_tok[0] = a1
                    if stage == 10:
                        dump(stat[:], [a1])
                    a2 = act(rs[:, tti:tti + 1], ssq[:, tti:tti + 1], AF.Sqrt, [a1], scale=1.0 / D, bias=EPS)
                    if stage == 11:
                        dump(stat[:], [a2])
                    v1 = P.op("dve", lambda e, o=rstd[:, tti:tti + 1], i=rs[:, tti:tti + 1]: e.reciprocal(out=o, in_=i), [a2])
                    v2 = stt("dve", hn[k], xin, rstd[:, tti:tti + 1], gbc[:], ALU.mult, ALU.mult, [v1, gt, hnfree[k], ltok])
                    if stage == 12:
                        dump(stat[:], [v1])
                    if stage == 13:
                        dump(cD[:, 8192:12288].bitcast(F32), [v2])
                    xfree[k] = [a1, v2]
                    tps = []
                    for g in range(4):
                        rk, pt, fr = tp_ring.get()
                        tl = None
                        for j in range(4):
                            c = g * 4 + j
                            tl = tpose(pt[:, j, :], hn[k][:, c * 128:(c + 1) * 128], ident_bf[:],
                                       [v2, fr, ctok] if j == 0 else (), sig=(j == 3))
                        eng = "act"
                        cp = vcopy(eng, dstT[:, g * 4:(g + 1) * 4, tti * 128:(tti + 1) * 128], pt, [tl, extra_deps])
                        if stage == 16 + g:
                            dump(stat[:], [cp])
                        tp_ring.release(rk, [cp])
                        tps.append(tl)
                        last.append(cp)
                    hnfree[k] = tps
                    if stage == 14:
                        dump(arenaA[:, 0:8192].bitcast(F32), last)
                return last

            def load_ctx(tti, k, free):
                tok = dma("sp", xt[k], xc[tti * 128:(tti + 1) * 128, :], xs[k], free)
                return xt[k], tok

            hT_ready = norm_transpose(None, 16, ssq1, rs1, rstd1, hT, gtok, load_ctx)
            p1_done = list(hT_ready)
            if stage == 1:
                dump(arenaA[:].bitcast(F32), p1_done)

            whfree = [[], []]
            wh_tok = {}
            selz = P.op("dve", lambda e: e.memset(selbT, 0.0), [p1_done])

            def load_wh(h):
                k = head_list.index(h) % 2
                deps_ = whfree[k]
                sl_ = newslot(f"wh_{h}")
                wh_tok[h] = dma("pool", wh[k].rearrange("p c n -> p (c n)"), w_in_h[h], sl_, deps_)

            head_list = list(range(NH))
            if stage == 5:
                head_list = [8]
            for h_ in head_list[:2]:
                load_wh(h_)
            qkv_free = list(p1_done)
            acc_free = [[], []]
            small_free = []
            onT_written = []
            ssq_tok = {0: t0, 1: t0}
            evac_i = [0]
            last_proj_mm = None

            ring = mm_ring
            for hi_, h in enumerate(head_list):
                moba = h < 8
                k = hi_ % 2
                if (not moba) and ring is mm_ring:
                    ring = Ring(ps_mm + [ps_acc[1]])
                    ring.free = [list(mm_ring.free[0]), list(mm_ring.free[1]), list(mm_ring.free[2]), [acc_free[1], onT_written[-6:]]]
                W = wh[k]
                wtok = wh_tok[h]
                head_reads = []
                proj_evac = []
                for which, ntile, dst, col0, sc in (("q", 2, QT, 0, SCALE), ("k", 4, KT, 128, 1.0)):
                    for ti in range(ntile):
                        pos0 = (HALF + ti * 512) if which == "q" else ti * 512
                        rk, pm, fr = ring.get()
                        for c in range(16):
                            lt = mm(pm[:], W[:, c, col0:col0 + 128], hT[:, c, pos0:pos0 + 512], c == 0, c == 15,
                                    [wtok, fr, hT_ready] if c == 0 else (), sig=(c == 15))
                        dsl = dst[:, ti * 512:(ti + 1) * 512]
                        if moba:
                            e1 = act(raw32, pm[0:32, :], AF.Copy, [lt, qkv_free], scale=sc)
                            if stage == 20:
                                dump(stat[:], [e1])
                            h1 = vcopy("dve", rhi, raw32, [e1, qkv_free])
                            h2 = tt("dve", rlo, raw32, rhi, ALU.subtract, [h1])
                            rk2, pm2, fr2 = ring.get()
                            mm(pm2[:], perm[:], rhi, True, False, [h1, fr2, ctok])
                            sw = mm(pm2[:], perm[:], rlo, False, True, [h2], sig=True)
                            d1 = tt("dve", ropeA, raw32, ropeC[:, pos0:pos0 + 512], ALU.mult, [e1, ctok])
                            d2 = tt("dve", ropeB, pm2[0:32, :], ropeS[:, pos0:pos0 + 512], ALU.mult, [sw, ctok])
                            if stage == 21:
                                dump(stat[:], [d1, d2])
                            e2 = act(dsl, pm[:], AF.Copy, [lt, qkv_free], scale=sc)
                            d3 = tt("dve", dsl[0:32, :], ropeA, ropeB, ALU.add, [d1, d2, e2, qkv_free])
                            ring.release(rk2, [d2])
                            ring.release(rk, [e1, e2])
                            proj_evac += [d3, e2]
                            qkv_free = [qkv_free, d3]
                        else:
                            evac_i[0] += 1
                            eng = "act" if evac_i[0] % 2 == 0 else "dve"
                            if eng == "act":
                                e2 = act(dsl, pm[:], AF.Copy, [lt, qkv_free], scale=sc)
                            else:
                                e2 = ts("dve", dsl, pm[:], sc, ALU.mult, deps=[lt, qkv_free])
                            ring.release(rk, [e2])
                            proj_evac.append(e2)
                if stage == 23:
                    dump(cD[:, 0:5120].bitcast(F32), [proj_evac])
                for g in range(4):
                    rk, pm, fr = ring.get()
                    for j in range(4):
                        tti = g * 4 + j
                        for c in range(16):
                            lt = mm(pm[:, j * 128:(j + 1) * 128], hT[:, c, tti * 128:(tti + 1) * 128], W[:, c, 256:384],
                                    c == 0, c == 15, [wtok, fr, hT_ready] if (j == 0 and c == 0) else (),
                                    sig=(j == 3 and c == 15))
                    evac_i[0] += 1
                    eng = "act" if evac_i[0] % 2 == 0 else "dve"
                    if stage == 24:
                        eng = "act"
                    if stage == 25:
                        eng = "dve"
                    e2 = vcopy(eng, Vt[:, g * 4:(g + 1) * 4, :], pm[:].rearrange("p (t d) -> p t d", t=4), [lt, qkv_free])
                    if stage in (24, 25) and g == 1:
                        dump(cD[:, 0:5120].bitcast(F32), [proj_evac, e2])
                    if stage == 26 and g == 2:
                        dump(cD[:, 0:5120].bitcast(F32), [proj_evac, e2])
                    if stage == 27 and g == 3:
                        dump(cD[:, 0:5120].bitcast(F32), [proj_evac, e2])
                    ring.release(rk, [e2])
                    proj_evac.append(e2)
                last_proj_mm = lt
                whfree[k] = [lt]
                if stage in (2, 28, 29) and h == 0:
                    dump(cD[:, 0:5120].bitcast(F32), [proj_evac])
                if hi_ + 2 < len(head_list):
                    load_wh(head_list[hi_ + 2])

                if moba:
                    r1 = P.op("dve", lambda e: e.tensor_reduce(out=km, in_=KT.rearrange("p (n k) -> p n k", n=8),
                                                               axis=AX.X, op=ALU.add), [proj_evac])
                    r2 = ts("dve", kms, km, 1.0 / 256.0, ALU.mult, deps=[r1])
                    r3 = vcopy("dve", km_hi, kms, [r2])
                    r4 = tt("dve", km_lo, kms, km_hi, ALU.subtract, [r3])
                    for j in range(8):
                        mm(ps_small[:, j * 8:(j + 1) * 8], QT[:, j * 128:(j + 1) * 128], km_hi, True, False,
                           [r4, proj_evac, small_free] if j == 0 else ())
                        gl = mm(ps_small[:, j * 8:(j + 1) * 8], QT[:, j * 128:(j + 1) * 128], km_lo, False, True, sig=(j == 7))
                    g1 = tt("dve", gm, ps_small[:, 0:64], gbias[:], ALU.add, [gl, ctok])
                    small_free = [g1]
                    gx = g1
                    for j in range(8):
                        gx = P.op("dve", lambda e, o=top[:, j * 8:(j + 1) * 8], i=gm[:, j * 8:(j + 1) * 8]: e.max(out=o, in_=i), [gx])
                    for j in range(8):
                        gx = ts("dve", sel[:, j * 8:(j + 1) * 8], gm[:, j * 8:(j + 1) * 8], top[:, j * 8 + 3:j * 8 + 4],
                                ALU.is_gt, deps=[gx])
                    gx = tt("dve", sel, sel, ownb[:], ALU.add, [gx])
                    gx = ts("dve", selb[:], sel, -1.0, ALU.add, NEGBIG, ALU.mult, deps=[gx])
                    sel_cp = []
                    for half_ in range(2):
                        rk, pt, fr = tp_ring.get()
                        for j in range(4):
                            jj = half_ * 4 + j
                            tl = tpose(pt[0:8, j, :], selb[:, jj * 8:(jj + 1) * 8], ident_bf[:],
                                       [gx, fr, ctok] if j == 0 else (), sig=(j == 3))
                        cp = act(selbT[0:8, half_ * 512:(half_ + 1) * 512], pt[0:8].rearrange("p j t -> p (j t)"), AF.Copy,
                                 [tl, qkv_free, selz])
                        tp_ring.release(rk, [cp])
                        sel_cp.append(cp)

                if stage == 3 and h == 0:
                    dump(gsm[:].rearrange("p a b -> p (a b)"), [gx, sel_cp])
                for Qi in range(2):
                    qs = QT[:, Qi * 512:(Qi + 1) * 512]
                    qsl = slice(Qi * 512, (Qi + 1) * 512)
                    last_kt = 8 + 4 * Qi + 3
                    accO = ps_acc[0]
                    accD = ps_acc[1]
                    d0 = 8 + 4 * Qi
                    if moba:
                        kts = list(range(0, last_kt + 1))
                        n = len(kts)
                        st_, pv_tok = {}, {}

                        def A_(idx):
                            kt = kts[idx]
                            r = idx % 2
                            rk, pm, fr = ring.get()
                            mm(pm[:], KT[:, kt * 128:(kt + 1) * 128], qs, True, False, [proj_evac, fr, sel_cp])
                            s1 = mm(pm[:], onehot[:, kt // 2, :], selbT[:, qsl], False, True, sig=True)
                            e1 = act(Ab[r], pm[:], AF.Exp, [s1, pv_tok.get(idx - 2)])
                            ring.release(rk, [e1])
                            pt = e1
                            if kt >= d0:
                                pt = tt("dve", Ab[r], Ab[r], mask_mb[:, kt - d0, :], ALU.mult, [e1, ctok])
                            st_[idx] = pt

                        def B_(idx):
                            kt = kts[idx]
                            r = idx % 2
                            first = idx == 0
                            lastk = idx == n - 1
                            mm(accO[:], Vt[:, kt, :], Ab[r], first, lastk, [st_[idx], acc_free[0]] if first else [st_[idx]])
                            pv_tok[idx] = mm(accD[:], ones[:], Ab[r], first, lastk, [acc_free[1]] if first else (), sig=True)

                        A_(0)
                        for idx in range(n):
                            if idx + 1 < n:
                                A_(idx + 1)
                            B_(idx)
                        fin = pv_tok[n - 1]
                    else:
                        kts = list(range(last_kt, -1, -1))
                        n = len(kts)
                        ztok, zbuf, ltok_, gtok_, gbuf, rtoks, rbuf, xtok, vtok = {}, {}, {}, {}, {}, {}, {}, {}, {}

                        def Z_(idx):
                            kt = kts[idx]
                            rk, pm, fr = ring.get()
                            ztok[idx] = mm(pm[:], KT[:, kt * 128:(kt + 1) * 128], qs, True, True, [proj_evac, fr], sig=True)
                            zbuf[idx] = (rk, pm)

                        def E_(idx):
                            kt = kts[idx]
                            r = idx % 2
                            rk, pm = zbuf[idx]
                            e1 = act(Ef[r], pm[:], AF.Exp, [ztok[idx]])
                            ring.release(rk, [e1])
                            e2 = act(Lp[r], Ef[r], AF.Ln, [e1, gtok_.get(idx - 2), rtoks.get(idx - 2)], bias=1.0)
                            lt_ = e2
                            if kt >= d0:
                                lt_ = tt("dve", Lp[r], Lp[r], mask_sb[:, kt - d0, :], ALU.mult, [e2, ctok])
                            ltok_[idx] = lt_

                        def G_(idx):
                            kt = kts[idx]
                            r = idx % 2
                            first = idx == 0
                            rk2, pg, fr2 = ring.get()
                            mm(pg[:], KT[:, kt * 128:(kt + 1) * 128], qs, True, False, [fr2, proj_evac])
                            g2 = mm(pg[:], tri[:], Lp[r], False, first, [ltok_[idx], ctok], sig=first)
                            if not first:
                                g2 = mm(pg[:], negones[:], Rb[rbuf[idx - 1]], False, True, [rtoks[idx - 1]], sig=True)
                            gtok_[idx] = g2
                            gbuf[idx] = (rk2, pg)

                        def R_(idx):
                            if idx == n - 1:
                                return
                            r = idx % 2
                            if idx == 0:
                                rtoks[0] = vcopy("dve", Rb[0], Lp[r], [ltok_[0]])
                                rbuf[0] = 0
                            else:
                                nb = 1 - rbuf[idx - 1]
                                rtoks[idx] = tt("dve", Rb[nb], Rb[rbuf[idx - 1]], Lp[r], ALU.add,
                                                [ltok_[idx], rtoks[idx - 1], gtok_.get(idx - 1)])
                                rbuf[idx] = nb

                        def X_(idx):
                            kt = kts[idx]
                            r = idx % 2
                            rk2, pg = gbuf[idx]
                            e3 = act(Ab[r], pg[:], AF.Exp, [gtok_[idx], vtok.get(idx - 2)])
                            ring.release(rk2, [e3])
                            at_ = e3
                            if kt >= d0:
                                at_ = tt("dve", Ab[r], Ab[r], mask_sb[:, kt - d0, :], ALU.mult, [e3])
                            xtok[idx] = at_

                        def V_(idx):
                            kt = kts[idx]
                            r = idx % 2
                            first = idx == 0
                            lastk = idx == n - 1
                            vtok[idx] = mm(accO[:], Vt[:, kt, :], Ab[r], first, lastk,
                                           [xtok[idx], acc_free[0]] if first else [xtok[idx]], sig=True)

                        Z_(0)
                        Z_(1)
                        E_(0)
                        G_(0)
                        for idx in range(n):
                            if idx + 2 < n:
                                Z_(idx + 2)
                            if idx + 1 < n:
                                E_(idx + 1)
                            R_(idx)
                            if idx + 1 < n:
                                G_(idx + 1)
                            X_(idx)
                            V_(idx)
                        fin = vtok[n - 1]
                    head_reads = [fin]
                    if moba:
                        v1 = P.op("dve", lambda e: e.reciprocal(out=rden, in_=accD[:]), [fin, onT_written[-2:]])
                        v2 = tt("dve", Of, accO[:], rden, ALU.mult, [v1])
                        acc_free = [[v2], [v1]]
                    else:
                        v2 = vcopy("dve", Of, accO[:], [fin, onT_written[-2:]])
                        acc_free = [[v2], acc_free[1]]
                    a1 = act(onT[:, h, Qi * 512:(Qi + 1) * 512], Of, AF.Copy, [v2, ctok], scale=go_col[:, h:h + 1])
                    a2 = act(sqb, Of, AF.Square, [v2, onT_written[-2:]])
                    for j in range(4):
                        sl = mm(ps_small[:, 64 + j:65 + j], sqb[:, j * 128:(j + 1) * 128], ones[:, 0:1], True, True,
                                [a2, small_free] if j == 0 else (), sig=(j == 3))
                    gi = 0 if moba else 1
                    sacc = ssqA if moba else ssqB
                    v3 = tt("dve", sacc[:, Qi * 4:(Qi + 1) * 4], sacc[:, Qi * 4:(Qi + 1) * 4], ps_small[:, 64:68], ALU.add,
                            [sl, ssq_tok[gi]])
                    ssq_tok[gi] = v3
                    small_free = [small_free, v3]
                    onT_written += [a1, a2, sl]
                    head_reads = [head_reads, a1, a2]
                qkv_free = [head_reads, onT_written[-6:]]
                if (stage == 4 and h == 0) or (stage == 5 and h == 8):
                    dump(arenaB[:, h * 1024:(h + 1) * 1024].bitcast(F32), [onT_written, qkv_free, ssq_tok[0], ssq_tok[1]])

            att_done = [onT_written, ssq_tok[0], ssq_tok[1], qkv_free]
            if ring is not mm_ring:
                for kk_ in range(3):
                    mm_ring.free[kk_] = [ring.free[kk_], ring.free[3]]

            x1s = newslot("x1ld")
            x1tok = None
            for tti in range(8):
                x1tok = dma("sp", x1[:, tti, :], xc[HALF + tti * 128:HALF + (tti + 1) * 128, :], x1s, [last_proj_mm, att_done])
            g2tok = dma("sp", gbc[:], g2_bc, gslot, [p1_done, att_done])
            a_ = act(rsAB, stat[:, 48:64], AF.Sqrt, [att_done], scale=1.0 / 1024.0, bias=EPS)
            rAB = P.op("dve", lambda e: e.reciprocal(out=rstdAB, in_=rsAB), [a_])
            wofree = [[att_done], [att_done]]
            wo_tok = {}

            def load_wo(et):
                kk = et % 2
                sl_ = newslot(f"wo_{et}")
                for hh in range(2):
                    wo_tok[et] = dma("pool", wo[kk][:, hh * 8:(hh + 1) * 8, :].rearrange("p h e -> p (h e)"),
                                     w_out_r[et][:, hh * 4096:(hh + 1) * 4096], sl_, wofree[kk])

            load_wo(0)
            load_wo(1)
            x1_last = {}
            for et in range(4):
                kk = et % 2
                for tti in range(8):
                    toks = []
                    for gi in range(2):
                        rk, pm, fr = mm_ring.get()
                        for hh in range(8):
                            h = gi * 8 + hh
                            lt = mm(pm[:], onT[:, h, tti * 128:(tti + 1) * 128], wo[kk][:, h, :], hh == 0, hh == 7,
                                    [wo_tok[et], fr, att_done] if hh == 0 else (), sig=(hh == 7))
                        xsl = x1[:, tti, et * 512:(et + 1) * 512]
                        v = stt("dve", xsl, pm[:], rstdAB[:, gi * 8 + tti:gi * 8 + tti + 1], xsl, ALU.mult, ALU.add,
                                [lt, rAB, x1tok, x1_last.get((tti, et))])
                        x1_last[(tti, et)] = v
                        mm_ring.release(rk, [v])
                        toks.append(lt)
                wofree[kk] = [lt]
                if et + 2 < 4:
                    load_wo(et + 2)
            op_done = [x1_last[kk_] for kk_ in x1_last] + [lt]
            if stage == 6:
                dump(arenaA[:].bitcast(F32), op_done)

            def load_x1(tti, k, free):
                return x1[:, tti, :], None

            h2_ready = norm_transpose(None, 8, ssq2, rs2, rstd2, h2T, g2tok, load_x1, extra_deps=op_done)

            if stage == 7:
                dump(arenaB[:].bitcast(F32), h2_ready)
            wufree = [[op_done], [op_done]]
            wdfree = [[op_done]]
            wu_tok = {}
            wd_tok = {}

            def load_wu(fb):
                kk = fb % 2
                sl_ = newslot(f"wu_{fb}")
                for fc in range(4):
                    wu_tok[fb] = dma("pool", wu[kk][:, fc].rearrange("p c n -> p (c n)"),
                                     w_up_r[fb][:, fc * 2048:(fc + 1) * 2048], sl_, wufree[kk])

            def load_wd(fb):
                wd_tok[fb] = dma("pool", wd1, w_down[fb * 512:(fb + 1) * 512, :].rearrange("(c p) e -> p c e", p=128),
                                 newslot(f"wd_{fb}"), wdfree[0])

            load_wu(0)
            load_wd(0)
            load_wu(1)
            ATfree = [[], []]
            rlfree = [[], []]
            rli = 0
            for fb in range(16):
                kk = fb % 2
                at_w = []
                for fc in range(4):
                    for tq in range(2):
                        rk, pm, fr = mm_ring.get()
                        for c in range(16):
                            lt = mm(pm[:], wu[kk][:, fc, c, :], h2T[:, c, tq * 512:(tq + 1) * 512], c == 0, c == 15,
                                    [wu_tok[fb], fr, h2_ready] if c == 0 else (), sig=(c == 15))
                        ri = rli % 2
                        rli += 1
                        e1 = act(rls[ri][:], pm[:], AF.Relu, [lt, rlfree[ri]])
                        mm_ring.release(rk, [e1])
                        v = tt("dve", ATs[kk][:, fc, tq * 512:(tq + 1) * 512], rls[ri][:], rls[ri][:], ALU.mult, [e1, ATfree[kk]])
                        rlfree[ri] = [v]
                        at_w.append(v)
                wufree[kk] = [lt]
                if fb + 2 < 16:
                    load_wu(fb + 2)
                for tti in range(8):
                    for et in range(4):
                        rk, pm, fr = mm_ring.get()
                        for fc in range(4):
                            lt = mm(pm[:], ATs[kk][:, fc, tti * 128:(tti + 1) * 128], wd1[:, fc, et * 512:(et + 1) * 512],
                                    fc == 0, fc == 3, [wd_tok[fb], fr, at_w] if fc == 0 else (), sig=(fc == 3))
                        xsl = x1[:, tti, et * 512:(et + 1) * 512]
                        v = tt("dve", xsl, xsl, pm[:], ALU.add, [lt, x1_last.get((tti, et)), h2_ready])
                        x1_last[(tti, et)] = v
                        mm_ring.release(rk, [v])
                wdfree[0] = [lt]
                ATfree[kk] = [lt]
                if fb + 1 < 16:
                    load_wd(fb + 1)

            mlp_done = [x1_last[kk_] for kk_ in x1_last]
            gftok = dma("sp", gbc[:], gf_bc, gslot, [h2_ready])
            osl = [newslot("os0"), newslot("os1")]
            ofree = [[mlp_done], [mlp_done]]
            store_toks = []
            for tti in range(8):
                k = tti % 2
                a1 = act(junk, x1[:, tti, :], AF.Square, [mlp_done, junk_tok[0]], accum_out=ssq3[:, tti:tti + 1])
                junk_tok[0] = a1
                a2 = act(rs3[:, tti:tti + 1], ssq3[:, tti:tti + 1], AF.Sqrt, [a1], scale=1.0 / D, bias=EPS)
                v1 = P.op("dve", lambda e, o=rstd3[:, tti:tti + 1], i=rs3[:, tti:tti + 1]: e.reciprocal(out=o, in_=i), [a2])
                v2 = stt("dve", yo[k], x1[:, tti, :], rstd3[:, tti:tti + 1], gbc[:], ALU.mult, ALU.mult, [v1, gftok, ofree[k]])
                st = dma("sp", out_d[tti * 128:(tti + 1) * 128, :], yo[k], osl[k], [v2])
                ofree[k] = [st]
                store_toks.append(st)
            P.op("sp", lambda e: e.nop(), store_toks, sig=False)

        try:
            plan()
        except _Stop:
            pass

        engmap = {"pe": "tensor", "act": "scalar", "dve": "vector", "pool": "gpsimd", "sp": "sync"}
        with nc.Block() as block:
            for ename in Prog.ENGS:
                def body(e, ename=ename):
                    for (waits, fn, inc) in P.q[ename]:
                        for (key, val) in waits:
                            e.wait_ge(sems[key], val)
                        ins = fn(e)
                        if inc is not None:
                            ins.then_inc(sems[inc[0]], inc[1])
                getattr(block, engmap[ename])(body)
    return nc


_NC_CACHE = {}


def _bf(a):
    return np.ascontiguousarray(a.astype(ml_dtypes.bfloat16))


def make_in_maps(x, mix_norm_g, w_in, moba_out_g, sb_out_g, w_out, mlp_norm_g, w_up, w_down, final_norm_g):
    x = np.asarray(x, dtype=np.float32)
    w_in = np.asarray(w_in, dtype=np.float32)[0]
    w_out = np.asarray(w_out, dtype=np.float32)[0]
    w_up = np.asarray(w_up, dtype=np.float32)[0]
    w_down_ = np.ascontiguousarray(np.asarray(w_down, dtype=np.float32)[0])
    B = x.shape[0]

    heads = []
    for h in range(NH):
        if h < 8:
            qc, kc, vc = h * 128, 1024 + h * 128, 2048 + h * 128
        else:
            hh = h - 8
            qc, kc, vc = 3072 + hh * 128, 4096 + hh * 128, 5120 + hh * 128
        sl = np.concatenate([w_in[:, qc:qc + 128], w_in[:, kc:kc + 128], w_in[:, vc:vc + 128]], axis=1)
        heads.append(sl.reshape(16, 128, 384).transpose(1, 0, 2).reshape(128, 16 * 384))
    w_in_h = np.ascontiguousarray(np.stack(heads, 0))
    w_out_r = np.ascontiguousarray(
        w_out.reshape(16, 128, 4, 512).transpose(2, 1, 0, 3).reshape(4, 128, 16 * 512))
    w_up_r = np.ascontiguousarray(
        w_up.reshape(16, 128, 16, 4, 128).transpose(2, 1, 3, 0, 4).reshape(16, 128, 4 * 16 * 128))

    def bc(v):
        return np.ascontiguousarray(np.broadcast_to(np.asarray(v, np.float32).reshape(1, D), (128, D)))

    g1 = bc(np.asarray(mix_norm_g)[0])
    g2 = bc(np.asarray(mlp_norm_g)[0])
    gf = bc(np.asarray(final_norm_g))
    go = np.concatenate([np.asarray(moba_out_g, np.float32)[0], np.asarray(sb_out_g, np.float32)[0]])
    go_col = np.ascontiguousarray(go.reshape(16, 128).T)

    kk = np.arange(128)[:, None]
    qq = np.arange(512)[None, :]
    msb = np.stack([((128 * j + kk) < qq) for j in range(4)], 1).astype(np.float32)
    mmb = np.stack([((128 * j + kk) <= qq) for j in range(4)], 1).astype(np.float32)
    mask_sb = _bf(msb.reshape(128, 2048))
    mask_mb = _bf(mmb.reshape(128, 2048))
    perm = np.zeros((32, 128), np.float32)
    for m in range(32):
        perm[(m + 16) % 32, m] = 1.0
    ident = np.eye(128, dtype=np.float32)
    tri = np.where(np.arange(128)[:, None] >= np.arange(128)[None, :], -1.0, 0.0).astype(np.float32)
    onehot = np.zeros((128, 8, 128), np.float32)
    for n in range(8):
        onehot[n, n, :] = 1.0
    inv_freq = (np.float32(500000.0) ** (-np.arange(16, dtype=np.float32) / np.float32(16))).astype(np.float32)

    in_maps = []
    for c in range(2 * B):
        b, half = c // 2, c % 2
        if half == 1:
            xcx = np.ascontiguousarray(x[b])
            pos = np.arange(S, dtype=np.float32)
        else:
            xcx = np.concatenate([np.zeros((HALF, D), np.float32), x[b, :HALF]], 0)
            pos = np.maximum(np.arange(S, dtype=np.float32) - HALF, 0).astype(np.float32)
        ang = pos[None, :] * inv_freq[:, None]
        cos = np.cos(ang).astype(np.float32)
        sin = np.sin(ang).astype(np.float32)
        ropeC = np.concatenate([cos, cos], 0)
        ropeS = np.concatenate([-sin, sin], 0)
        gb = np.zeros((128, 8, 8), np.float32)
        ob = np.zeros((128, 8, 8), np.float32)
        for j in range(8):
            qblk = 4 + j // 2
            for n in range(8):
                elig = (n < qblk) and (half == 1 or n >= 4)
                gb[:, j, n] = 0.0 if elig else -1e30
            ob[:, j, qblk] = 1.0
        in_maps.append({
            "xc": np.ascontiguousarray(xcx), "w_in_h": w_in_h, "w_out_r": w_out_r, "w_up_r": w_up_r, "w_down": w_down_,
            "g1_bc": g1, "g2_bc": g2, "gf_bc": gf, "go_col": go_col,
            "ropeC": np.ascontiguousarray(ropeC), "ropeS": np.ascontiguousarray(ropeS), "perm": _bf(perm),
            "mask_sb": mask_sb, "mask_mb": mask_mb,
            "gbias": np.ascontiguousarray(gb.reshape(128, 64)), "ownb": np.ascontiguousarray(ob.reshape(128, 64)),
            "ident_bf": _bf(ident), "tri": _bf(tri), "negones": _bf(-np.ones((128, 128), np.float32)),
            "ones": _bf(np.ones((128, 128), np.float32)), "onehot": _bf(onehot.reshape(128, 1024)),
        })

    return in_maps


def kernel(x, mix_norm_g, w_in, moba_out_g, sb_out_g, w_out, mlp_norm_g, w_up, w_down, final_norm_g):
    x = np.asarray(x, dtype=np.float32)
    B = x.shape[0]
    in_maps = make_in_maps(x, mix_norm_g, w_in, moba_out_g, sb_out_g, w_out, mlp_norm_g, w_up, w_down, final_norm_g)
    if "nc" not in _NC_CACHE:
        _NC_CACHE["nc"] = build_program()
    nc = _NC_CACHE["nc"]
    res = run_bass_kernel_spmd(nc, in_maps, core_ids=list(range(2 * B)))
    out = np.zeros((B, S, D), np.float32)
    for c in range(2 * B):
        b, half = c // 2, c % 2
        out[b, half * HALF:(half + 1) * HALF] = np.asarray(res.results[c]["out"], dtype=np.float32)
    return out
```
